# Optimizing a Trainium2 kernel written in Bass

```python
import math
import jax, jax.numpy as jnp
from jax import lax
import numpy as np

D_MODEL = 2048
BATCH = 8
SEQ = 2048
DEPTH = 2

GRID_W = 64
CTX_LEN = 256
Q_BLOCK = 128
ROPE_THETA = 10000.0
EPS = 1e-6
NEG_INF = -1e30

NA_HEADS = 4
NA_HEAD_DIM = 128
NA_KH_MAX = 8
NA_KW = 16
NA_QCB = 16
NA_KCB = 2 * NA_KW
MLA_HEADS = 4
MLA_NOPE = 128
MLA_ROPE = 64
MLA_V = 128
MLA_Q_RANK = 384
MLA_KV_RANK = 128
GQA_HEADS = 4
GQA_KV_HEADS = 2
GQA_HEAD_DIM = 128
DIFF_HEADS = 4
DIFF_QK_DIM = 64
DIFF_V_DIM = 128

NA_SCALE = NA_HEAD_DIM ** -0.5
MLA_SCALE = (MLA_NOPE + MLA_ROPE) ** -0.5
GQA_SCALE = GQA_HEAD_DIM ** -0.5
DIFF_SCALE = DIFF_QK_DIM ** -0.5

MIX_WIDTH = NA_HEADS * NA_HEAD_DIM + MLA_HEADS * MLA_V + GQA_HEADS * GQA_HEAD_DIM + DIFF_HEADS * DIFF_V_DIM
IN_SIZES = (NA_HEADS * NA_HEAD_DIM, NA_HEADS * NA_HEAD_DIM, NA_HEADS * NA_HEAD_DIM,
            MLA_Q_RANK, MLA_KV_RANK, MLA_ROPE,
            GQA_HEADS * GQA_HEAD_DIM, GQA_KV_HEADS * GQA_HEAD_DIM, GQA_KV_HEADS * GQA_HEAD_DIM,
            DIFF_HEADS * 2 * DIFF_QK_DIM, DIFF_HEADS * 2 * DIFF_QK_DIM, DIFF_HEADS * DIFF_V_DIM)
IN_COLS = sum(IN_SIZES)
IN_SPLIT_POINTS = tuple(int(v) for v in np.cumsum(IN_SIZES)[:-1])

D_FF = 5632
N_EXPERTS = 8
TOP_K = 2
D_FF_EXPERT = 7168

kernel_name = 'hybrid_parallel_heads_dit_block'


def rmsnorm(x, g):
    xf = x.astype(jnp.float32)
    y = xf * lax.rsqrt(jnp.mean(xf * xf, axis=-1, keepdims=True) + EPS)
    return y.astype(x.dtype) * g


def modulate(x, shift, scale):
    return x * (1.0 + scale) + shift


def split_heads(t, n_heads):
    return t.reshape(t.shape[:2] + (n_heads, t.shape[-1] // n_heads))


def axial_angles(n_tokens, dim):
    half = dim // 2
    freqs = ROPE_THETA ** (-jnp.arange(0, half, 2, dtype=jnp.float32) / half)
    t = jnp.arange(n_tokens, dtype=jnp.int32)
    row = (t // GRID_W).astype(jnp.float32)
    col = (t % GRID_W).astype(jnp.float32)
    ang = jnp.stack([row[:, None] * freqs, col[:, None] * freqs], axis=1)
    return jnp.cos(ang), jnp.sin(ang)


def rope_2d(x, cos, sin):
    q = x.shape[-1] // 4
    xr = x.reshape(x.shape[:-1] + (2, 2, q))
    shape = (1, x.shape[1]) + (1,) * (x.ndim - 3) + (2, q)
    c = cos.reshape(shape).astype(x.dtype)
    s = sin.reshape(shape).astype(x.dtype)
    x1, x2 = xr[..., 0, :], xr[..., 1, :]
    out = jnp.stack([x1 * c - x2 * s, x2 * c + x1 * s], axis=-2)
    return out.reshape(x.shape)


def over_query_blocks(fn, *qs):
    b, n = qs[0].shape[:2]
    nb = n // Q_BLOCK
    blocks = tuple(jnp.moveaxis(q.reshape((b, nb, Q_BLOCK) + q.shape[2:]), 1, 0) for q in qs)
    out = lax.map(lambda qb: fn(*qb), blocks)
    out = jnp.moveaxis(out, 0, 1)
    return out.reshape((b, n) + out.shape[3:])


def softmax_attend(q, k, v, scale):
    b, nq, h, dk = q.shape
    g = k.shape[2]
    qg = q.reshape(b, nq, g, h // g, dk)
    s = jnp.einsum('bqgrd,bkgd->bgrqk', qg, k) * scale
    p = jax.nn.softmax(s.astype(jnp.float32), axis=-1).astype(v.dtype)
    o = jnp.einsum('bgrqk,bkgv->bqgrv', p, v)
    return o.reshape(b, nq, h, v.shape[-1])


def diff_attend(q1, q2, k1, k2, v, lam, scale):
    s1 = jnp.einsum('bqhd,bkhd->bhqk', q1, k1) * scale
    s2 = jnp.einsum('bqhd,bkhd->bhqk', q2, k2) * scale
    p = jax.nn.softmax(s1.astype(jnp.float32), axis=-1) - lam * jax.nn.softmax(s2.astype(jnp.float32), axis=-1)
    return jnp.einsum('bhqk,bkhv->bqhv', p.astype(v.dtype), v)


def natten_attend(q, k, v, k_ctx, v_ctx, rel_bias):
    b, n, h, d = q.shape
    rows = n // GRID_W
    kh = min(NA_KH_MAX, rows)
    ncb = GRID_W // NA_QCB
    qc = np.arange(GRID_W).reshape(ncb, NA_QCB)
    kb = np.clip(np.arange(ncb) * NA_QCB - NA_KW // 2, 0, GRID_W - NA_KCB)
    kc = kb[:, None] + np.arange(NA_KCB)
    cs = np.clip(qc - NA_KW // 2, 0, GRID_W - NA_KW)
    col_ok = (kc[:, None, :] >= cs[:, :, None]) & (kc[:, None, :] < cs[:, :, None] + NA_KW)
    col_off = np.clip(kc[:, None, :] - qc[:, :, None] + NA_KW - 1, 0, 2 * NA_KW - 2)
    col_bias = rel_bias[:, :, col_off]
    mask = jnp.asarray(np.broadcast_to(col_ok[:, :, None, :], (ncb, NA_QCB, kh, NA_KCB)).reshape(ncb, NA_QCB, kh * NA_KCB))
    nk = kh * NA_KCB

    def one_row(r):
        rs = jnp.clip(r - kh // 2, 0, rows - kh)
        qr = lax.dynamic_slice_in_dim(q, r * GRID_W, GRID_W, axis=1).reshape(b, ncb, NA_QCB, h, d)

        def band(t):
            t = lax.dynamic_slice_in_dim(t, rs * GRID_W, kh * GRID_W, axis=1).reshape(b, kh, GRID_W, h, d)
            t = t[:, :, kc]
            return jnp.moveaxis(t, 2, 1).reshape(b, ncb, nk, h, d)

        kr, vr = band(k), band(v)
        row_idx = rs - r + jnp.arange(kh) + NA_KH_MAX - 1
        bias = jnp.take(col_bias, row_idx, axis=1)
        bias = jnp.transpose(bias, (0, 2, 3, 1, 4)).reshape(h, ncb, NA_QCB, nk).astype(jnp.float32)
        s_lat = jnp.einsum('bnqhd,bnkhd->bhnqk', qr, kr).astype(jnp.float32) * NA_SCALE + bias
        s_lat = jnp.where(mask, s_lat, NEG_INF)
        s_ctx = jnp.einsum('bnqhd,bkhd->bhnqk', qr, k_ctx).astype(jnp.float32) * NA_SCALE
        p = jax.nn.softmax(jnp.concatenate([s_lat, s_ctx], axis=-1), axis=-1).astype(v.dtype)
        o = (jnp.einsum('bhnqk,bnkhd->bnqhd', p[..., :nk], vr)
             + jnp.einsum('bhnqk,bkhd->bnqhd', p[..., nk:], v_ctx))
        return o.reshape(b, GRID_W, h, d)

    out = lax.map(one_row, jnp.arange(rows, dtype=jnp.int32))
    return jnp.moveaxis(out, 0, 1).reshape(b, n, h, d)


def mla_queries(cq, g_q, w_qup, rope):
    q = split_heads(rmsnorm(cq, g_q) @ w_qup, MLA_HEADS)
    q_nope, q_pe = q[..., :MLA_NOPE], q[..., MLA_NOPE:]
    if rope is not None:
        q_pe = rope_2d(q_pe, *rope)
    return jnp.concatenate([q_nope, q_pe], axis=-1)


def mla_keys_values(ckv, k_pe, g_kv, w_kvup, rope):
    kv = split_heads(rmsnorm(ckv, g_kv) @ w_kvup, MLA_HEADS)
    k_nope, v = kv[..., :MLA_NOPE], kv[..., MLA_NOPE:]
    k_pe = k_pe[:, :, None, :]
    if rope is not None:
        k_pe = rope_2d(k_pe, *rope)
    k_pe = jnp.broadcast_to(k_pe, k_nope.shape[:-1] + (MLA_ROPE,))
    return jnp.concatenate([k_nope, k_pe], axis=-1), v


def gqa_qk(t, n_heads, g, rope):
    z = rmsnorm(split_heads(t, n_heads), g)
    if rope is not None:
        z = rope_2d(z, *rope)
    return z


def diff_qk(t, rope):
    z = t.reshape(t.shape[:2] + (DIFF_HEADS, 2, DIFF_QK_DIM))
    if rope is not None:
        z = rope_2d(z, *rope)
    return z[..., 0, :], z[..., 1, :]


def diff_lambda(lq1, lk1, lq2, lk2, lam_init):
    e = lambda a, b: jnp.exp(jnp.sum(a.astype(jnp.float32) * b.astype(jnp.float32)))
    return e(lq1, lk1) - e(lq2, lk2) + lam_init


def merge_heads(o_na, o_mla, o_gqa, o_diff, g_sub, lam_init, w_out):
    o_diff = rmsnorm(o_diff, g_sub) * (1.0 - lam_init)
    b, n = o_na.shape[:2]
    o = jnp.concatenate([o_na.reshape(b, n, -1), o_mla.reshape(b, n, -1),
                         o_gqa.reshape(b, n, -1), o_diff.reshape(b, n, -1)], axis=-1)
    return o @ w_out


def token_mixer(u, uc, lp, lam_init, rope64, rope128, need_ctx):
    (na_q, na_k, na_v, mla_cq, mla_ckv, mla_kpe, gqa_q, gqa_k, gqa_v,
     diff_q, diff_k, diff_v) = jnp.split(u @ lp['w_in'], IN_SPLIT_POINTS, axis=-1)
    (na_q_ctx, na_k_ctx, na_v_ctx, mla_cq_ctx, mla_ckv_ctx, mla_kpe_ctx, gqa_q_ctx, gqa_k_ctx, gqa_v_ctx,
     diff_q_ctx, diff_k_ctx, diff_v_ctx) = jnp.split(uc @ lp['w_in'], IN_SPLIT_POINTS, axis=-1)
    cat = lambda a, b: jnp.concatenate([a, b], axis=1)

    k_na_ctx, v_na_ctx = split_heads(na_k_ctx, NA_HEADS), split_heads(na_v_ctx, NA_HEADS)
    o_na = natten_attend(split_heads(na_q, NA_HEADS), split_heads(na_k, NA_HEADS), split_heads(na_v, NA_HEADS),
                         k_na_ctx, v_na_ctx, lp['na_rel_bias'])
    k_mla_ctx, v_mla_ctx = mla_keys_values(mla_ckv_ctx, mla_kpe_ctx, lp['mla_g_kv'], lp['mla_w_kvup'], None)
    k_mla, v_mla = mla_keys_values(mla_ckv, mla_kpe, lp['mla_g_kv'], lp['mla_w_kvup'], rope64)
    k_mla, v_mla = cat(k_mla_ctx, k_mla), cat(v_mla_ctx, v_mla)
    q_mla = mla_queries(mla_cq, lp['mla_g_q'], lp['mla_w_qup'], rope64)
    o_mla = over_query_blocks(lambda q: softmax_attend(q, k_mla, v_mla, MLA_SCALE), q_mla)
    k_gqa_ctx = gqa_qk(gqa_k_ctx, GQA_KV_HEADS, lp['gqa_g_k'], None)
    v_gqa_ctx = split_heads(gqa_v_ctx, GQA_KV_HEADS)
    k_gqa = cat(k_gqa_ctx, gqa_qk(gqa_k, GQA_KV_HEADS, lp['gqa_g_k'], rope128))
    v_gqa = cat(v_gqa_ctx, split_heads(gqa_v, GQA_KV_HEADS))
    q_gqa = gqa_qk(gqa_q, GQA_HEADS, lp['gqa_g_q'], rope128)
    o_gqa = over_query_blocks(lambda q: softmax_attend(q, k_gqa, v_gqa, GQA_SCALE), q_gqa)
    lam = diff_lambda(lp['diff_lq1'], lp['diff_lk1'], lp['diff_lq2'], lp['diff_lk2'], lam_init)
    k1_ctx, k2_ctx = diff_qk(diff_k_ctx, None)
    v_diff_ctx = split_heads(diff_v_ctx, DIFF_HEADS)
    k1, k2 = diff_qk(diff_k, rope64)
    k1, k2 = cat(k1_ctx, k1), cat(k2_ctx, k2)
    v_diff = cat(v_diff_ctx, split_heads(diff_v, DIFF_HEADS))
    q1, q2 = diff_qk(diff_q, rope64)
    o_diff = over_query_blocks(lambda a, b: diff_attend(a, b, k1, k2, v_diff, lam, DIFF_SCALE), q1, q2)
    out = merge_heads(o_na, o_mla, o_gqa, o_diff, lp['diff_g_sub'], lam_init, lp['w_out'])
    if not need_ctx:
        return out, None

    o_na_ctx = over_query_blocks(lambda q: softmax_attend(q, k_na_ctx, v_na_ctx, NA_SCALE), split_heads(na_q_ctx, NA_HEADS))
    q_mla_ctx = mla_queries(mla_cq_ctx, lp['mla_g_q'], lp['mla_w_qup'], None)
    o_mla_ctx = over_query_blocks(lambda q: softmax_attend(q, k_mla_ctx, v_mla_ctx, MLA_SCALE), q_mla_ctx)
    q_gqa_ctx = gqa_qk(gqa_q_ctx, GQA_HEADS, lp['gqa_g_q'], None)
    o_gqa_ctx = over_query_blocks(lambda q: softmax_attend(q, k_gqa_ctx, v_gqa_ctx, GQA_SCALE), q_gqa_ctx)
    q1_ctx, q2_ctx = diff_qk(diff_q_ctx, None)
    o_diff_ctx = over_query_blocks(lambda a, b: diff_attend(a, b, k1_ctx, k2_ctx, v_diff_ctx, lam, DIFF_SCALE), q1_ctx, q2_ctx)
    out_ctx = merge_heads(o_na_ctx, o_mla_ctx, o_gqa_ctx, o_diff_ctx, lp['diff_g_sub'], lam_init, lp['w_out'])
    return out, out_ctx


def swiglu(u, w_gate, w_up, w_down):
    return (jax.nn.silu(u @ w_gate) * (u @ w_up)) @ w_down


def moe_swiglu(u, w_router, b_router, w_gate, w_up, w_down):
    b, n, d = u.shape
    t = u.reshape(b * n, d)
    logits = (t @ w_router).astype(jnp.float32) + b_router.astype(jnp.float32)
    top_val, top_idx = lax.top_k(logits, TOP_K)
    gates = jax.nn.softmax(top_val, axis=-1)
    comb = jnp.sum(jax.nn.one_hot(top_idx, N_EXPERTS, dtype=jnp.float32) * gates[..., None], axis=1).astype(u.dtype)
    y = jnp.zeros_like(t)
    for e in range(N_EXPERTS):
        y = y + comb[:, e:e + 1] * swiglu(t, w_gate[e], w_up[e], w_down[e])
    return y.reshape(b, n, d)


def setup_inputs(seed: int = 0) -> dict:
    key = jax.random.key(seed)
    keys = jax.random.split(key, 32)
    n_dense = (DEPTH + 1) // 2
    n_moe = DEPTH // 2
    D = D_MODEL

    def nrm(i, shape, scale):
        return scale * jax.random.normal(keys[i], shape, jnp.float32)

    def gain(i, shape):
        return 1.0 + nrm(i, shape, 0.1)

    return {
        'x': nrm(0, (BATCH, SEQ, D), 1.0),
        'c': nrm(1, (BATCH, D), 1.0),
        'ctx': nrm(2, (BATCH, CTX_LEN, D), 1.0),
        'c_ctx': nrm(3, (D,), 1.0),
        'w_mod': nrm(4, (DEPTH, D, 6 * D), 0.3 * D ** -0.5),
        'b_mod': nrm(5, (DEPTH, 6 * D), 0.02),
        'g_mix': gain(6, (DEPTH, D)),
        'w_in': nrm(7, (DEPTH, D, IN_COLS), D ** -0.5),
        'na_rel_bias': nrm(8, (DEPTH, NA_HEADS, 2 * NA_KH_MAX - 1, 2 * NA_KW - 1), 0.3),
        'mla_g_q': gain(9, (DEPTH, MLA_Q_RANK)),
        'mla_w_qup': nrm(10, (DEPTH, MLA_Q_RANK, MLA_HEADS * (MLA_NOPE + MLA_ROPE)), MLA_Q_RANK ** -0.5),
        'mla_g_kv': gain(11, (DEPTH, MLA_KV_RANK)),
        'mla_w_kvup': nrm(12, (DEPTH, MLA_KV_RANK, MLA_HEADS * (MLA_NOPE + MLA_V)), MLA_KV_RANK ** -0.5),
        'gqa_g_q': gain(13, (DEPTH, GQA_HEAD_DIM)),
        'gqa_g_k': gain(14, (DEPTH, GQA_HEAD_DIM)),
        'diff_lq1': nrm(15, (DEPTH, DIFF_QK_DIM), 0.1),
        'diff_lk1': nrm(16, (DEPTH, DIFF_QK_DIM), 0.1),
        'diff_lq2': nrm(17, (DEPTH, DIFF_QK_DIM), 0.1),
        'diff_lk2': nrm(18, (DEPTH, DIFF_QK_DIM), 0.1),
        'diff_g_sub': gain(19, (DEPTH, DIFF_V_DIM)),
        'w_out': nrm(20, (DEPTH, MIX_WIDTH, D), MIX_WIDTH ** -0.5),
        'g_ffn': gain(21, (DEPTH, D)),
        'ffn_w_gate': nrm(22, (n_dense, D, D_FF), D ** -0.5),
        'ffn_w_up': nrm(23, (n_dense, D, D_FF), D ** -0.5),
        'ffn_w_down': nrm(24, (n_dense, D_FF, D), D_FF ** -0.5),
        'moe_w_router': nrm(25, (n_moe, D, N_EXPERTS), D ** -0.5),
        'moe_b_router': nrm(26, (n_moe, N_EXPERTS), 0.01),
        'moe_w_gate': nrm(27, (n_moe, N_EXPERTS, D, D_FF_EXPERT), D ** -0.5),
        'moe_w_up': nrm(28, (n_moe, N_EXPERTS, D, D_FF_EXPERT), D ** -0.5),
        'moe_w_down': nrm(29, (n_moe, N_EXPERTS, D_FF_EXPERT, D), D_FF_EXPERT ** -0.5),
        'g_final': gain(30, (D,)),
    }


def reference(x, c, ctx, c_ctx, w_mod, b_mod, g_mix, w_in, na_rel_bias, mla_g_q, mla_w_qup, mla_g_kv, mla_w_kvup,
              gqa_g_q, gqa_g_k, diff_lq1, diff_lk1, diff_lq2, diff_lk2, diff_g_sub, w_out, g_ffn,
              ffn_w_gate, ffn_w_up, ffn_w_down, moe_w_router, moe_b_router, moe_w_gate, moe_w_up, moe_w_down,
              g_final):
    n = x.shape[1]
    rope64 = axial_angles(n, MLA_ROPE)
    rope128 = axial_angles(n, GQA_HEAD_DIM)
    s_c = jax.nn.silu(c)
    s_cc = jax.nn.silu(c_ctx)
    h, hc = x, ctx
    for l in range(DEPTH):
        last = l == DEPTH - 1
        mod = (s_c @ w_mod[l] + b_mod[l])[:, None, :]
        sh_a, sc_a, gt_a, sh_f, sc_f, gt_f = jnp.split(mod, 6, axis=-1)
        csh_a, csc_a, cgt_a, csh_f, csc_f, cgt_f = jnp.split(s_cc @ w_mod[l] + b_mod[l], 6, axis=-1)
        lp = {'w_in': w_in[l], 'w_out': w_out[l], 'na_rel_bias': na_rel_bias[l],
              'mla_g_q': mla_g_q[l], 'mla_w_qup': mla_w_qup[l], 'mla_g_kv': mla_g_kv[l], 'mla_w_kvup': mla_w_kvup[l],
              'gqa_g_q': gqa_g_q[l], 'gqa_g_k': gqa_g_k[l],
              'diff_lq1': diff_lq1[l], 'diff_lk1': diff_lk1[l], 'diff_lq2': diff_lq2[l], 'diff_lk2': diff_lk2[l],
              'diff_g_sub': diff_g_sub[l]}
        lam_init = 0.8 - 0.6 * math.exp(-0.3 * l)
        u = modulate(rmsnorm(h, g_mix[l]), sh_a, sc_a)
        uc = modulate(rmsnorm(hc, g_mix[l]), csh_a, csc_a)
        a, a_ctx = token_mixer(u, uc, lp, lam_init, rope64, rope128, not last)
        h = h + gt_a * a
        u = modulate(rmsnorm(h, g_ffn[l]), sh_f, sc_f)
        if l % 2 == 0:
            ffn = lambda z: swiglu(z, ffn_w_gate[l // 2], ffn_w_up[l // 2], ffn_w_down[l // 2])
        else:
            ffn = lambda z: moe_swiglu(z, moe_w_router[l // 2], moe_b_router[l // 2], moe_w_gate[l // 2],
                                       moe_w_up[l // 2], moe_w_down[l // 2])
        h = h + gt_f * ffn(u)
        if not last:
            hc = hc + cgt_a * a_ctx
            uc = modulate(rmsnorm(hc, g_ffn[l]), csh_f, csc_f)
            hc = hc + cgt_f * ffn(uc)
    return rmsnorm(h, g_final)
```

```python
import math
import numpy as np
import concourse.bass as bass
import concourse.mybir as mybir
from concourse.bass_utils import run_bass_kernel_spmd

F32 = mybir.dt.float32
BF16 = mybir.dt.bfloat16
AF = mybir.ActivationFunctionType
ALU = mybir.AluOpType

D = 2048
SEQ = 2048
CTX = 256
NTOK = SEQ + CTX
NT = NTOK // 128
KC = D // 128
EPS = 1e-6
GRID_W = 64
DFF = 5632
NEXP = 8
DFFE = 7168
IN_COLS = 4672
OFF = dict(na_q=0, na_k=512, na_v=1024, mla_cq=1536, mla_ckv=1920, mla_kpe=2048, gqa_q=2112, gqa_k=2624,
           gqa_v=2880, diff_q=3136, diff_k=3648, diff_v=4160)
NA_SCALE = 128 ** -0.5
MLA_SCALE = 192 ** -0.5
GQA_SCALE = 128 ** -0.5
DIFF_SCALE = 64 ** -0.5


class Prog:
    NDSEM = 12

    def __init__(self, nc, same_engine_sync=True):
        self.nc = nc
        self.eng = {"pe": nc.tensor, "act": nc.scalar, "dve": nc.vector, "pool": nc.gpsimd, "sp": nc.sync}
        self.csem = {e: nc.alloc_semaphore(name=f"cs_{e}") for e in ("pe", "act", "dve", "pool")}
        self.ccnt = {e: 0 for e in self.csem}
        self.dsem = {q: [nc.alloc_semaphore(name=f"ds_{q}{i}") for i in range(self.NDSEM)]
                     for q in ("sp", "pool")}
        self.dcnt = {q: 0 for q in self.dsem}
        self.waited = {}
        self.res = {}
        self.same_engine_sync = same_engine_sync
        self.n_inst = 0
        self.phase = "init"
        self.relaxed = True
        self.phase_log = []

    def set_phase(self, name):
        self.phase = name
        self.phase_log.append((name, self.ccnt["pe"]))

    def _sem(self, sk):
        return self.csem[sk[1]] if sk[0] == "c" else self.dsem[sk[1]][sk[2]]

    def _wait(self, e, tok, raw=True):
        sk, v = tok
        if sk[0] == "c" and sk[1] == e and (e == "pe" or not self.same_engine_sync or (self.relaxed and not raw)):
            return
        key = (e, sk)
        if self.waited.get(key, 0) >= v:
            return
        self.waited[key] = v
        self.eng[e].wait_ge(self._sem(sk), v)

    def _deps(self, reads, writes):
        deps = []
        for r in reads:
            st = self.res.get(r)
            if st and st[0] is not None:
                deps.append((st[0], True))
        for w in writes:
            st = self.res.get(w)
            if st:
                if st[0] is not None:
                    deps.append((st[0], False))
                deps.extend((it, False) for it in st[1].items())
        return deps

    def _record(self, tok, reads, writes):
        sk, v = tok
        for r in reads:
            st = self.res.setdefault(r, [None, {}])
            if st[1].get(sk, 0) < v:
                st[1][sk] = v
        for w in writes:
            self.res[w] = [tok, {}]

    def op(self, e, fn, reads=(), writes=()):
        for d, raw in self._deps(reads, writes):
            self._wait(e, d, raw)
        inst = fn(self.eng[e])
        inst.then_inc(self.csem[e], 1)
        self.ccnt[e] += 1
        self.n_inst += 1
        tok = (("c", e), self.ccnt[e])
        self._record(tok, reads, writes)
        return tok

    def dma(self, q, out, in_, reads=(), writes=(), **kw):
        i = self.dcnt[q]
        slot, rnd = i % self.NDSEM, i // self.NDSEM
        for d, raw in self._deps(reads, writes):
            self._wait(q, d, True)
        if rnd > 0:
            self._wait(q, (("d", q, slot), 16 * rnd))
        inst = self.eng[q].dma_start(out=out, in_=in_, **kw)
        inst.then_inc(self.dsem[q][slot], 16)
        self.dcnt[q] += 1
        self.n_inst += 1
        tok = (("d", q, slot), 16 * (rnd + 1))
        self._record(tok, reads, writes)
        return tok

    def barrier(self, engines=("pe", "act", "dve", "pool", "sp")):
        toks = [(("c", x), n) for x, n in self.ccnt.items() if n > 0]
        for q, n in self.dcnt.items():
            for s in range(min(n, self.NDSEM)):
                toks.append((("d", q, s), 16 * ((n - 1 - s) // self.NDSEM + 1)))
        for e in engines:
            for t in toks:
                sk, v = t
                if sk[0] == "c" and sk[1] == e:
                    continue
                key = (e, sk)
                if self.waited.get(key, 0) >= v:
                    continue
                self.waited[key] = v
                self.eng[e].wait_ge(self._sem(sk), v)
        if len(engines) == 5:
            self.res = {}


class _Stop(Exception):
    pass


class Arena:
    def __init__(self, nc, limit):
        self.nc, self.off, self.limit, self.n = nc, 18560, limit, 0

    def alloc(self, name, shape, dt):
        nbytes = int(np.prod(shape[1:])) * (2 if dt == BF16 else 4)
        nbytes = (nbytes + 63) // 64 * 64
        assert self.off + nbytes <= self.limit, (name, self.off, nbytes)
        self.n += 1
        t = self.nc.alloc_sbuf_tensor_at(f"{name}_{self.n}", list(shape), dt, offset=self.off)
        self.off += nbytes
        return t


class Ring:
    def __init__(self, A, name, n, shape, dt):
        self.name = name
        self.t = [A.alloc(f"{name}{i}", shape, dt) for i in range(n)]
        self.i = 0

    def next(self):
        k = self.i % len(self.t)
        self.i += 1
        return self.t[k], (self.name, k)


def build_program(dbg=None):
    nc = bass.Bass("TRN2", target_bir_lowering=False)
    P = Prog(nc)
    din = lambda n, s, dt=F32: nc.dram_tensor(n, list(s), dt, kind="ExternalInput")
    x = din("x", [SEQ, D]).ap()
    ctx = din("ctx", [CTX, D]).ap()
    c_in = din("c", [1, D]).ap()
    cc_in = din("c_ctx", [1, D]).ap()
    w_mod = din("w_mod", [2, D, 6 * D]).ap()
    b_mod = din("b_mod", [2, 6 * D]).ap()
    g_mix = din("g_mix", [2, D]).ap()
    w_in = din("w_in", [2, D, IN_COLS]).ap()
    na_bias = din("na_bias_exp", [2, 4, 15, 64, 64]).ap()
    mla_g_q = din("mla_g_q", [2, 384]).ap()
    mla_w_qup = din("mla_w_qup", [2, 384, 768]).ap()
    mla_g_kv = din("mla_g_kv", [2, 128]).ap()
    mla_w_kvup = din("mla_w_kvup", [2, 128, 1024]).ap()
    gqa_g_q = din("gqa_g_q", [2, 128]).ap()
    gqa_g_k = din("gqa_g_k", [2, 128]).ap()
    dlam = din("diff_lam", [2, 4, 64]).ap()
    diff_g_sub = din("diff_g_sub", [2, 128]).ap()
    w_out = din("w_out", [2, D, D]).ap()
    g_ffn = din("g_ffn", [2, D]).ap()
    ffn_wg = din("ffn_w_gate", [1, D, DFF]).ap()
    ffn_wu = din("ffn_w_up", [1, D, DFF]).ap()
    ffn_wd = din("ffn_w_down", [1, DFF, D]).ap()
    moe_wr = din("moe_w_router", [1, D, NEXP]).ap()
    moe_br = din("moe_b_router", [1, NEXP]).ap()
    moe_wg = din("moe_w_gate", [1, NEXP, D, DFFE]).ap()
    moe_wu = din("moe_w_up", [1, NEXP, D, DFFE]).ap()
    moe_wd = din("moe_w_down", [1, NEXP, DFFE, D]).ap()
    g_final = din("g_final", [1, D]).ap()
    c_ident = din("c_ident", [128, 128]).ap()
    c_perm16 = din("c_perm16", [128, 128]).ap()
    c_perm32 = din("c_perm32", [128, 128]).ap()
    c_rope = din("c_rope", [4, 128, SEQ]).ap()
    c_cmask = din("c_cmask", [128, 64]).ap()
    out = nc.dram_tensor("out", [SEQ, D], F32, kind="ExternalOutput").ap()
    dbg_out = {}

    def dbg_tensor(name, shape, dt=F32):
        t = nc.dram_tensor("dbg_" + name, list(shape), dt, kind="ExternalOutput").ap()
        dbg_out[name] = t
        return t

    h_scr = nc.dram_tensor("h_scr", [NTOK, D], F32, kind="Internal").ap()
    modv = nc.dram_tensor("modv", [2, 2, 6 * D], F32, kind="Internal").ap()
    o_scr = nc.dram_tensor("o_scr", [16, 128, NTOK], BF16, kind="Internal").ap()
    u2_scr = nc.dram_tensor("u2_scr", [16, 128, NTOK], BF16, kind="Internal").ap()

    A = Arena(nc, 229000)
    pa = nc.alloc_psum_tensor("pa", [128, 2048], F32)
    pb = nc.alloc_psum_tensor("pb", [128, 2048], F32)
    pa_b = pa[:].bitcast(BF16)
    PA = lambda j, r=128, n=512: pa[0:r, j * 512:j * 512 + n]
    PB = lambda j, r=128, n=512: pb[0:r, j * 512:j * 512 + n]

    identf = A.alloc("identf", [128, 128], F32)
    identb = A.alloc("identb", [128, 128], BF16)
    onesb = A.alloc("onesb", [128, 128], BF16)
    onesf = A.alloc("onesf", [128, 128], F32)
    ones1 = A.alloc("ones1", [128, 128], F32)
    epsb = A.alloc("epsb", [128, 1], F32)
    comb = A.alloc("comb", [128, 16, NEXP], F32)
    P.dma("sp", identf[:], c_ident, writes=["identf"])
    P.op("dve", lambda e: e.tensor_copy(out=identb[:], in_=identf[:]), reads=["identf"], writes=["identb"])
    P.op("dve", lambda e: e.memset(onesb[:], 1.0), writes=["onesb"])
    P.op("dve", lambda e: e.memset(onesf[:], 1.0 / 128), writes=["onesf"])
    P.op("dve", lambda e: e.memset(ones1[:], 1.0), writes=["ones1"])
    P.op("dve", lambda e: e.memset(epsb[:], EPS), writes=["epsb"])
    CONST_END = A.off

    def bcast_row(dst, src_row, key, q="sp"):
        return P.dma(q, dst, src_row.partition_broadcast(128), writes=[key])

    def phase_mod():
        P.set_phase("mod")
        base = A.off
        cT = A.alloc("cT", [128, 16, 2], F32)
        sT = A.alloc("sT", [128, 16, 2], F32)
        wm = Ring(A, "wm", 3, [128, 16, 512], F32)
        bmr = Ring(A, "bmr", 3, [1, 512], F32)
        mo = Ring(A, "mo", 3, [2, 512], F32)
        with nc.allow_non_contiguous_dma(reason="tiny transposed load of c"):
            P.dma("sp", cT[:, :, 0], c_in.rearrange("o (k p) -> p (o k)", p=128), writes=["cT0"])
            P.dma("sp", cT[:, :, 1], cc_in.rearrange("o (k p) -> p (o k)", p=128), writes=["cT1"])
        P.op("act", lambda e: e.activation(out=sT[:], in_=cT[:], func=AF.Silu), reads=["cT0", "cT1"], writes=["sT"])
        for l in range(2):
            wsrc = w_mod[l].rearrange("(k p) n -> p k n", p=128)
            for cb in range(24):
                wt, wk = wm.next()
                P.dma("sp", wt[:], wsrc[:, :, cb * 512:(cb + 1) * 512], writes=[wk])
                bt, bk = bmr.next()
                P.dma("sp", bt[:], b_mod[l:l + 1, cb * 512:(cb + 1) * 512], writes=[bk])
                for k in range(16):
                    P.op("pe", lambda e: e.matmul(PA(cb % 2, 2), lhsT=sT[:, k, :], rhs=wt[:, k, :], start=(k == 0), stop=False),
                         reads=["sT", wk], writes=[("pa", cb % 2)])
                P.op("pe", lambda e: e.matmul(PA(cb % 2, 2), lhsT=ones1[0:1, 0:2], rhs=bt[:], start=False, stop=True),
                     reads=["ones1", bk], writes=[("pa", cb % 2)])
                mt, mk = mo.next()
                P.op("act", lambda e: e.copy(out=mt[:], in_=PA(cb % 2, 2)), reads=[("pa", cb % 2)], writes=[mk])
                P.dma("pool", modv[l, :, cb * 512:(cb + 1) * 512], mt[:], reads=[mk], writes=[("modv", l)])
        P.barrier()
        A.off = base

    def make_norm_bufs(nr=2):
        nb = dict(
            xt=Ring(A, "n_xt", 2, [128, D], F32),
            y1=Ring(A, "n_y1", nr, [128, D], F32),
            ub=Ring(A, "n_ub", nr, [128, D], BF16),
            junk=A.alloc("n_junk", [128, D], BF16),
            ss=Ring(A, "n_ss", 4, [128, 2], F32),
        )
        return nb

    tcount = [0]

    def norm_core(nb, xt, xk, G, Gk, Bt, Bk, dstT, dst_key, col0, want_f32T=None, defer=False):
        sst, ssk = nb["ss"].next()
        P.op("dve", lambda e: e.scalar_tensor_tensor(out=nb["junk"][:], in0=xt[:], scalar=1.0, in1=xt[:], op0=ALU.mult,
                                                      op1=ALU.mult, accum_out=sst[:, 0:1]),
             reads=[xk], writes=["n_junk", ssk])
        P.op("act", lambda e: e.activation(out=sst[:, 1:2], in_=sst[:, 0:1], func=AF.Sqrt, bias=epsb[:, 0:1], scale=1.0 / D),
             reads=[ssk, "epsb"], writes=[ssk])
        P.op("dve", lambda e: e.reciprocal(out=sst[:, 1:2], in_=sst[:, 1:2]), reads=[ssk], writes=[ssk])
        y1, y1k = nb["y1"].next()
        P.op("dve", lambda e: e.scalar_tensor_tensor(out=y1[:], in0=xt[:], scalar=sst[:, 1:2], in1=G[:], op0=ALU.mult, op1=ALU.mult),
             reads=[xk, ssk, Gk], writes=[y1k])
        i = tcount[0] % 2
        tcount[0] += 1
        if want_f32T is None:
            ub, ubk = nb["ub"].next()
            if Bt is not None:
                P.op("pool", lambda e: e.tensor_tensor(out=ub[:], in0=y1[:], in1=Bt[:], op=ALU.add), reads=[y1k, Bk], writes=[ubk])
            else:
                P.op("pool", lambda e: e.tensor_copy(out=ub[:], in_=y1[:]), reads=[y1k], writes=[ubk])
            pt = pa_b[:, i * 2048:(i + 1) * 2048].rearrange("p (c n) -> p c n", c=16)
            pk = [("pa", 2 * i), ("pa", 2 * i + 1)]

            def partB():
                for j in range(16):
                    P.op("pe", lambda e: e.transpose(pt[:, j, :], ub[:, j * 128:(j + 1) * 128], identb[:]),
                         reads=[ubk, "identb"], writes=[pk[j // 8]])
                P.op("act", lambda e: e.copy(out=dstT[:, :, col0:col0 + 128], in_=pt), reads=pk, writes=[dst_key])
        else:
            f32T, f32k = want_f32T
            if Bt is not None:
                P.op("pool", lambda e: e.tensor_tensor(out=y1[:], in0=y1[:], in1=Bt[:], op=ALU.add), reads=[y1k, Bk], writes=[y1k])

            def partB():
                for hh in range(4):
                    for j in range(4):
                        jj = hh * 4 + j
                        P.op("pe", lambda e: e.transpose(PA(hh)[:, j * 128:(j + 1) * 128], y1[:, jj * 128:(jj + 1) * 128], identf[:]),
                             reads=[y1k, "identf"], writes=[("pa", hh)])
                P.op("act", lambda e: e.copy(out=f32T[:].rearrange("p c n -> p (c n)"), in_=pa[:]), reads=[("pa", j) for j in range(4)], writes=[f32k])
                P.op("pool", lambda e: e.tensor_copy(out=dstT[:, :, col0:col0 + 128], in_=f32T[:]), reads=[f32k], writes=[dst_key])

        if defer:
            return partB
        partB()

    def h_src(l, t):
        if l == 0:
            return (ctx[t * 128:(t + 1) * 128, :] if t < 2 else x[(t - 2) * 128:(t - 1) * 128, :]), None
        return h_scr[t * 128:(t + 1) * 128, :], ("h", t)

    def load_mod_tiles(l, names, with_g=None):
        tiles = {}
        for who in (0, 1):
            for (slot, kind) in names:
                t = A.alloc(f"mt{who}{slot}", [128, D], F32)
                key = ("mt", who, slot)
                bcast_row(t[:], modv[l, who:who + 1, slot * D:(slot + 1) * D], key)
                tiles[(who, slot)] = (t, key)
        return tiles

    def layer(l):
        last = l == 1
        lam_init = 0.8 - 0.6 * math.exp(-0.3 * l)
        lay_base = A.off
        uT = A.alloc("uT", [128, 16, NTOK], BF16)

        P.set_phase(f"L{l}.N1")
        base = A.off
        nb = make_norm_bufs()
        gm = A.alloc("gm", [128, D], F32)
        bcast_row(gm[:], g_mix[l:l + 1, :], "gm")
        mt = load_mod_tiles(l, [(0, "sh"), (1, "sc")])
        for who in (0, 1):
            sc, sck = mt[(who, 1)]
            P.op("dve", lambda e: e.scalar_tensor_tensor(out=sc[:], in0=sc[:], scalar=1.0, in1=gm[:], op0=ALU.add, op1=ALU.mult),
                 reads=[sck, "gm"], writes=[sck])
        for t in range(NT):
            who = 1 if t < 2 else 0
            xt, xk = nb["xt"].next()
            src, sk = h_src(l, t)
            P.dma("sp", xt[:], src, reads=[sk] if sk else [], writes=[xk])
            norm_core(nb, xt, xk, mt[(who, 1)][0], mt[(who, 1)][1], mt[(who, 0)][0], mt[(who, 0)][1], uT, ("uT", t), t * 128)
        P.barrier()
        A.off = base
        if dbg == f"uT{l}":
            d = dbg_tensor("uT", [128, 16, NTOK], BF16)
            P.dma("sp", d, uT[:], reads=[("uT", t) for t in range(NT)], writes=["dbg"])
            raise _Stop()

        uT_keys = [("uT", t) for t in range(NT)]
        lat_blocks = [(CTX + 512 * i, 512) for i in range(4)]
        ctx_blocks = [(0, CTX)]
        q_blocks = lat_blocks + ([] if last else ctx_blocks)
        all_blocks = ctx_blocks + lat_blocks
        win = w_in[l].rearrange("(k p) n -> p k n", p=128)

        base = A.off
        wring = Ring(A, "wr", 2, [128, 16, 384], BF16)
        qT = Ring(A, "qT", 2, [128, NTOK], BF16)
        kT = Ring(A, "kT", 2, [128, NTOK], BF16)
        vv = Ring(A, "vv", 2, [128, NT, 128], BF16)
        Er = Ring(A, "E", 3, [128, 2, 512], BF16)
        e0r = Ring(A, "e0r", 2, [128, 2, 128], F32)
        f32r = Ring(A, "f32r", 6, [128, 512], F32)
        ofm = Ring(A, "ofm", 3, [128, 512], BF16)
        gcol = A.alloc("gcol", [128, 8], F32)
        pbi = [0]

        acnt = [0]

        def next_pb():
            j = pbi[0] % 4
            pbi[0] += 1
            return j

        def load_w(cols):
            wt, wk = wring.next()
            o = 0
            offs = []
            for (c0, n) in cols:
                P.dma("pool", wt[:, :, o:o + n], win[:, :, c0:c0 + n], writes=[wk])
                offs.append(o)
                o += n
            return wt, wk, offs

        def proj_fm(wt, wk, o, n, tok0, ntok):
            j = next_pb()
            for k in range(16):
                P.op("pe", lambda e: e.matmul(PB(j, n, ntok), lhsT=wt[:, k, o:o + n], rhs=uT[:, k, tok0:tok0 + ntok],
                                              start=(k == 0), stop=(k == 15)),
                     reads=[wk] + uT_keys[tok0 // 128:(tok0 + ntok) // 128], writes=[("pb", j)])
            return j

        def proj_v(wt, wk, o, vt, vk):
            for t in range(NT):
                j = next_pb()
                for k in range(16):
                    P.op("pe", lambda e: e.matmul(PB(j, 128, 128), lhsT=uT[:, k, t * 128:(t + 1) * 128], rhs=wt[:, k, o:o + 128],
                                                  start=(k == 0), stop=(k == 15)),
                         reads=[wk, ("uT", t)], writes=[("pb", j)])
                P.op("act", lambda e: e.copy(out=vt[:, t, :], in_=PB(j, 128, 128)), reads=[("pb", j)], writes=[(vk, t)])

        def mean_rstd(src_ap, src_keys, n_part, ntok, nchunks=1, denom=128.0):
            sq, sqk = f32r.next()
            rs, rsk = f32r.next()
            j = next_pb()
            for c in range(nchunks):
                s_ap = src_ap(c)
                P.op("dve", lambda e: e.tensor_tensor(out=sq[0:n_part, 0:ntok], in0=s_ap, in1=s_ap, op=ALU.mult), reads=src_keys, writes=[sqk])
                P.op("pe", lambda e: e.matmul(PB(j, 128, ntok), lhsT=ones1[0:n_part, :], rhs=sq[0:n_part, 0:ntok], start=(c == 0), stop=(c == nchunks - 1)),
                     reads=[sqk, "ones1"], writes=[("pb", j)])
            P.op("act", lambda e: e.activation(out=rs[:, 0:ntok], in_=PB(j, 128, ntok), func=AF.Sqrt, bias=epsb[:, 0:1], scale=1.0 / denom),
                 reads=[("pb", j), "epsb"], writes=[rsk])
            P.op("dve", lambda e: e.reciprocal(out=rs[:, 0:ntok], in_=rs[:, 0:ntok]), reads=[rsk], writes=[rsk])
            return rs, rsk

        rope_t = {}

        def rope_apply(z, zk, n_part, tok0, ntok, tab, perm, permk, dst_ap, dst_key):
            C, S = rope_t[tab]
            n0 = tok0 - CTX
            j = next_pb()
            P.op("pe", lambda e: e.matmul(PB(j, n_part, ntok), lhsT=perm[0:n_part, 0:n_part], rhs=z[0:n_part, 0:ntok], start=True, stop=True),
                 reads=[zk, permk], writes=[("pb", j)])
            t1, t1k = f32r.next()
            P.op("pool", lambda e: e.tensor_tensor(out=t1[0:n_part, 0:ntok], in0=z[0:n_part, 0:ntok], in1=C[0:n_part, n0:n0 + ntok], op=ALU.mult),
                 reads=[zk, "rope"], writes=[t1k])
            t2, t2k = f32r.next()
            P.op("dve", lambda e: e.tensor_tensor(out=t2[0:n_part, 0:ntok], in0=PB(j, n_part, ntok), in1=S[0:n_part, n0:n0 + ntok], op=ALU.mult),
                 reads=[("pb", j), "rope"], writes=[t2k])
            P.op("dve", lambda e: e.tensor_tensor(out=dst_ap, in0=t1[0:n_part, 0:ntok], in1=t2[0:n_part, 0:ntok], op=ALU.add),
                 reads=[t1k, t2k], writes=[dst_key])

        def attend(q_parts, k_parts, vt, vk, q0, nq, key_tiles, scale, masks=None):
            nk = len(key_tiles)
            par = acnt[0] % 2
            acnt[0] += 1
            OB = PB(2 * par, 128, nq)
            DB = PB(2 * par + 1, 128, nq)
            obk = ("pb", 2 * par)
            dbk = ("pb", 2 * par + 1)
            pairs = [key_tiles[i:i + 2] for i in range(0, nk, 2)]
            npr = len(pairs)
            pa3 = pa[:].rearrange("p (b n) -> p b n", b=4)

            def emit_S(pi_):
                b0 = 2 * (pi_ % 2)
                for idx, kt in enumerate(pairs[pi_]):
                    for pi, ((qt_, qk_, p0, p1), (kt_, kk_, _, _)) in enumerate(zip(q_parts, k_parts)):
                        P.op("pe", lambda e: e.matmul(PA(b0 + idx, 128, nq), lhsT=kt_[p0:p1, kt * 128:(kt + 1) * 128], rhs=qt_[p0:p1, q0:q0 + nq],
                                                      start=(pi == 0), stop=(pi == len(q_parts) - 1)),
                             reads=[qk_(q0, nq), kk_(kt)], writes=[("pa", b0 + idx)])

            emit_S(0)
            step = 0
            for pi_, pr in enumerate(pairs):
                if pi_ + 1 < npr:
                    emit_S(pi_ + 1)
                b0 = 2 * (pi_ % 2)
                n2 = len(pr)
                src = pa3[:, b0:b0 + n2, 0:nq]
                skeys = [("pa", b0 + idx) for idx in range(n2)]
                Et, Ek = Er.next()
                masked = masks is not None and pr[0] in masks
                if masked:
                    e0, e0k = e0r.next()
                    P.op("act", lambda e: e.activation(out=e0[:, 0:n2, 0:nq], in_=src, func=AF.Exp, scale=scale), reads=skeys, writes=[e0k])
                    for idx, kt in enumerate(pr):
                        W, Wk = masks[kt]
                        P.op("pool", lambda e: e.tensor_tensor(out=Et[:, idx, 0:nq], in0=e0[:, idx, 0:nq], in1=W, op=ALU.mult), reads=[e0k, Wk], writes=[Ek])
                else:
                    P.op("act", lambda e: e.activation(out=Et[:, 0:n2, 0:nq], in_=src, func=AF.Exp, scale=scale), reads=skeys, writes=[Ek])
                for idx, kt in enumerate(pr):
                    P.op("pe", lambda e: e.matmul(OB, lhsT=vt[:, kt, :], rhs=Et[:, idx, 0:nq], start=(step == 0), stop=(step == nk - 1)),
                         reads=[(vk, kt), Ek], writes=[obk])
                    P.op("pe", lambda e: e.matmul(DB, lhsT=onesb[:], rhs=Et[:, idx, 0:nq], start=(step == 0), stop=(step == nk - 1)),
                         reads=["onesb", Ek], writes=[dbk])
                    step += 1
            rd, rdk = f32r.next()
            P.op("dve", lambda e: e.reciprocal(out=rd[:, 0:nq], in_=DB), reads=[dbk], writes=[rdk])
            o, ok = f32r.next()
            P.op("dve", lambda e: e.tensor_tensor(out=o[:, 0:nq], in0=OB, in1=rd[:, 0:nq], op=ALU.mult), reads=[obk, rdk], writes=[ok])
            return o, ok

        def emit_o(chunk, src_ap, src_keys, q0, nq, scale_col=None):
            ot, otk = ofm.next()
            if scale_col is None:
                P.op("act", lambda e: e.copy(out=ot[:, 0:nq], in_=src_ap), reads=src_keys, writes=[otk])
            P.dma("sp", o_scr[chunk, :, q0:q0 + nq], ot[:, 0:nq], reads=[otk], writes=[("o", chunk, q0)])

        def tok_keys(name):
            return lambda q0, nq: (name, q0 // 128)

        whole = lambda key: (lambda *a: key)

        def is_lat(tok0):
            return tok0 >= CTX

        def mixer_na():
            P.set_phase(f"L{l}.na.setup")
            b0 = A.off
            Gexp = A.alloc("Gexp", [128, 4, 15, 64], F32)
            cmask = A.alloc("cmask", [128, 64], F32)
            Wt = A.alloc("Wt", [128, 24, 128], F32)
            P.dma("sp", cmask[:], c_cmask, writes=["cmask"])
            for a in range(2):
                P.dma("sp", Gexp[a * 64:(a + 1) * 64], na_bias[l].rearrange("h r k q -> k h r q"), writes=["Gexp"])
            Gf = Gexp[:].rearrange("p h r q -> p (h r) q")
            P.op("act", lambda e: e.activation(out=Gexp[:].rearrange("p h r q -> p (h r q)"), in_=Gexp[:].rearrange("p h r q -> p (h r q)"), func=AF.Exp),
                 reads=["Gexp"], writes=["Gexp"])
            P.op("dve", lambda e: e.tensor_tensor(out=Gf, in0=Gf, in1=cmask[:].unsqueeze(1).to_broadcast([128, 60, 64]), op=ALU.mult),
                 reads=["Gexp", "cmask"], writes=["Gexp"])
            rows = SEQ // GRID_W
            rs_of = lambda r: min(max(r - 4, 0), rows - 8)
            for h in range(4):
                P.set_phase(f"L{l}.na.proj")
                wt, wk, offs = load_w([(OFF["na_q"] + h * 128, 128), (OFF["na_k"] + h * 128, 128), (OFF["na_v"] + h * 128, 128)])
                q_t, qk = qT.next()
                k_t, kk = kT.next()
                v_t, vk = vv.next()
                for (t0, n) in all_blocks:
                    for (dst, dk, o) in ((q_t, qk, offs[0]), (k_t, kk, offs[1])):
                        if last and dst is q_t and not is_lat(t0):
                            continue
                        j = proj_fm(wt, wk, o, 128, t0, n)
                        P.op("act", lambda e: e.copy(out=dst[:, t0:t0 + n], in_=PB(j, 128, n)), reads=[("pb", j)], writes=[dk])
                P.set_phase(f"L{l}.na.v")
                proj_v(wt, wk, offs[2], v_t, vk)
                P.set_phase(f"L{l}.na.att")
                wcache = {}
                nW = [0]

                def get_W(r0, k0):
                    blocks = []
                    for a in range(2):
                        for b in range(2):
                            kr, r = k0 + a, r0 + b
                            ok = rs_of(r) <= kr < rs_of(r) + 8
                            blocks.append((kr - r + 7) if ok else None)
                    sig = tuple(blocks)
                    if sig in wcache:
                        return wcache[sig]
                    idx = nW[0]
                    nW[0] += 1
                    assert idx < 24
                    key = ("Wt", idx)
                    if any(bk is None for bk in blocks):
                        P.op("pool", lambda e: e.memset(Wt[:, idx, :], 0.0), writes=[key])
                    for a in range(2):
                        for b in range(2):
                            dr = blocks[a * 2 + b]
                            if dr is None:
                                continue
                            P.op("pool", lambda e: e.tensor_copy(out=Wt[a * 64:(a + 1) * 64, idx, b * 64:(b + 1) * 64],
                                                                 in_=Gexp[a * 64:(a + 1) * 64, h, dr, :]),
                                 reads=["Gexp"], writes=[key])
                    wcache[sig] = (Wt[:, idx, :], key)
                    return wcache[sig]

                qp = [(q_t, whole(qk), 0, 128)]
                kp = [(k_t, whole(kk), 0, 128)]
                for qt_i in range(16):
                    r0 = 2 * qt_i
                    lo = min(rs_of(r0), rs_of(r0 + 1))
                    hi = max(rs_of(r0), rs_of(r0 + 1)) + 8
                    k0s = list(range(lo - (lo % 2), hi, 2))
                    masks = {2 + k0 // 2: get_W(r0, k0) for k0 in k0s}
                    key_tiles = [0, 1] + [2 + k0 // 2 for k0 in k0s]
                    o, ok = attend(qp, kp, v_t, vk, CTX + qt_i * 128, 128, key_tiles, NA_SCALE, masks)
                    emit_o(h, o[:, 0:128], [ok], CTX + qt_i * 128, 128)
                if not last:
                    o, ok = attend(qp, kp, v_t, vk, 0, CTX, [0, 1], NA_SCALE)
                    emit_o(h, o[:, 0:CTX], [ok], 0, CTX)
            P.barrier()
            A.off = b0

        def load_rope(which):
            i0 = 0 if which == 128 else 2
            ts = []
            for i in range(2):
                t = A.alloc(f"rope{which}_{i}", [128, SEQ], F32)
                P.dma("sp", t[:], c_rope[i0 + i], writes=["rope"])
                ts.append(t)
            rope_t[which] = tuple(ts)
            pm = A.alloc(f"perm{which}", [128, 128], F32)
            P.dma("sp", pm[:], c_perm32 if which == 128 else c_perm16, writes=["perm32" if which == 128 else "perm16"])
            return pm

        def load_gcol(col, src_row_ap, n):
            with nc.allow_non_contiguous_dma(reason="tiny per-partition gain column"):
                P.dma("sp", gcol[0:n, col:col + 1], src_row_ap.rearrange("o n -> n o"), writes=[("gcol", col)])

        def mixer_gqa(p16, p32):
            P.set_phase(f"L{l}.gqa.proj")
            load_gcol(0, gqa_g_q[l:l + 1, :], 128)
            load_gcol(1, gqa_g_k[l:l + 1, :], 128)

            def normed_roped(wt, wk, o, gcolidx, dst, dk, t0, n):
                j = proj_fm(wt, wk, o, 128, t0, n)
                raw, rk = f32r.next()
                P.op("act", lambda e: e.copy(out=raw[:, 0:n], in_=PB(j, 128, n)), reads=[("pb", j)], writes=[rk])
                rs, rsk = mean_rstd(lambda c: raw[:, 0:n], [rk], 128, n)
                z, zk = f32r.next()
                P.op("dve", lambda e: e.scalar_tensor_tensor(out=z[:, 0:n], in0=raw[:, 0:n], scalar=gcol[:, gcolidx:gcolidx + 1], in1=rs[:, 0:n],
                                                              op0=ALU.mult, op1=ALU.mult), reads=[rk, rsk, ("gcol", gcolidx)], writes=[zk])
                if is_lat(t0):
                    rope_apply(z, zk, 128, t0, n, 128, p32, "perm32", dst[:, t0:t0 + n], dk)
                else:
                    P.op("act", lambda e: e.copy(out=dst[:, t0:t0 + n], in_=z[:, 0:n]), reads=[zk], writes=[dk])

            for g in range(2):
                wt, wk, offs = load_w([(OFF["gqa_k"] + g * 128, 128), (OFF["gqa_v"] + g * 128, 128)])
                k_t, kk = kT.next()
                v_t, vk = vv.next()
                for (t0, n) in all_blocks:
                    normed_roped(wt, wk, offs[0], 1, k_t, kk, t0, n)
                P.set_phase(f"L{l}.gqa.v")
                proj_v(wt, wk, offs[1], v_t, vk)
                for hh in range(2):
                    h = 2 * g + hh
                    wq, wqk, qoffs = load_w([(OFF["gqa_q"] + h * 128, 128)])
                    q_t, qk = qT.next()
                    P.set_phase(f"L{l}.gqa.proj")
                    for (t0, n) in q_blocks:
                        normed_roped(wq, wqk, qoffs[0], 0, q_t, qk, t0, n)
                    P.set_phase(f"L{l}.gqa.att")
                    qp = [(q_t, whole(qk), 0, 128)]
                    kp = [(k_t, whole(kk), 0, 128)]
                    for (q0, nq) in q_blocks:
                        kts = list(range(NT)) if is_lat(q0) else [0, 1]
                        o, ok = attend(qp, kp, v_t, vk, q0, nq, kts, GQA_SCALE)
                        emit_o(8 + h, o[:, 0:nq], [ok], q0, nq)

        def mixer_diff(p16, p32):
            P.set_phase(f"L{l}.diff.proj")
            load_gcol(2, diff_g_sub[l:l + 1, :], 128)
            lamt = A.alloc("lamt", [128, 4, 64], F32)
            lamc = A.alloc("lamc", [128, 4], F32)
            P.dma("sp", lamt[:].rearrange("p a n -> p (a n)"), dlam[l:l + 1].rearrange("o a n -> o (a n)").partition_broadcast(128), writes=["lamt"])
            for i in range(2):
                P.op("dve", lambda e: e.scalar_tensor_tensor(out=lamt[:, 2 * i, :], in0=lamt[:, 2 * i, :], scalar=1.0, in1=lamt[:, 2 * i + 1, :],
                                                              op0=ALU.mult, op1=ALU.mult, accum_out=lamc[:, i:i + 1]),
                     reads=["lamt"], writes=["lamt", "lamc"])
            P.op("act", lambda e: e.activation(out=lamc[:, 0:2], in_=lamc[:, 0:2], func=AF.Exp), reads=["lamc"], writes=["lamc"])
            P.op("dve", lambda e: e.tensor_tensor(out=lamc[:, 2:3], in0=lamc[:, 1:2], in1=lamc[:, 0:1], op=ALU.subtract), reads=["lamc"], writes=["lamc"])
            P.op("dve", lambda e: e.tensor_scalar(out=lamc[:, 2:3], in0=lamc[:, 2:3], scalar1=-lam_init, scalar2=None, op0=ALU.add), reads=["lamc"], writes=["lamc"])
            P.op("dve", lambda e: e.tensor_scalar(out=gcol[:, 3:4], in0=gcol[:, 2:3], scalar1=(1.0 - lam_init), scalar2=None, op0=ALU.mult),
                 reads=[("gcol", 2)], writes=[("gcol", 3)])
            for h in range(4):
                P.set_phase(f"L{l}.diff.proj")
                wt, wk, offs = load_w([(OFF["diff_q"] + h * 128, 128), (OFF["diff_k"] + h * 128, 128), (OFF["diff_v"] + h * 128, 128)])
                q_t, qk = qT.next()
                k_t, kk = kT.next()
                v_t, vk = vv.next()
                for (t0, n) in all_blocks:
                    for (dst, dk, o) in ((q_t, qk, offs[0]), (k_t, kk, offs[1])):
                        if last and dst is q_t and not is_lat(t0):
                            continue
                        j = proj_fm(wt, wk, o, 128, t0, n)
                        if is_lat(t0):
                            z, zk = f32r.next()
                            P.op("act", lambda e: e.copy(out=z[:, 0:n], in_=PB(j, 128, n)), reads=[("pb", j)], writes=[zk])
                            rope_apply(z, zk, 128, t0, n, 64, p16, "perm16", dst[:, t0:t0 + n], dk)
                        else:
                            P.op("act", lambda e: e.copy(out=dst[:, t0:t0 + n], in_=PB(j, 128, n)), reads=[("pb", j)], writes=[dk])
                P.set_phase(f"L{l}.diff.v")
                proj_v(wt, wk, offs[2], v_t, vk)
                P.set_phase(f"L{l}.diff.att")
                for (q0, nq) in q_blocks:
                    kts = list(range(NT)) if is_lat(q0) else [0, 1]
                    o1, o1k = attend([(q_t, whole(qk), 0, 64)], [(k_t, whole(kk), 0, 64)], v_t, vk, q0, nq, kts, DIFF_SCALE)
                    o2, o2k = attend([(q_t, whole(qk), 64, 128)], [(k_t, whole(kk), 64, 128)], v_t, vk, q0, nq, kts, DIFF_SCALE)
                    od, odk = f32r.next()
                    P.op("dve", lambda e: e.scalar_tensor_tensor(out=od[:, 0:nq], in0=o2[:, 0:nq], scalar=lamc[:, 2:3], in1=o1[:, 0:nq],
                                                                  op0=ALU.mult, op1=ALU.add), reads=[o1k, o2k, "lamc"], writes=[odk])
                    rs, rsk = mean_rstd(lambda c: od[:, 0:nq], [odk], 128, nq)
                    ot, otk = ofm.next()
                    P.op("dve", lambda e: e.scalar_tensor_tensor(out=ot[:, 0:nq], in0=od[:, 0:nq], scalar=gcol[:, 3:4], in1=rs[:, 0:nq],
                                                                  op0=ALU.mult, op1=ALU.mult), reads=[odk, rsk, ("gcol", 3)], writes=[otk])
                    P.dma("sp", o_scr[12 + h, :, q0:q0 + nq], ot[:, 0:nq], reads=[otk], writes=[("o", 12 + h, q0)])

        def mixer_mla(p16, p32):
            P.set_phase(f"L{l}.mla.proj")
            b0 = A.off
            cqn = A.alloc("cqn", [128, 3, NTOK], BF16)
            ckvn = A.alloc("ckvn", [128, NTOK], BF16)
            kpe = A.alloc("kpe", [64, NTOK], BF16)
            qpe = Ring(A, "qpe", 1, [64, NTOK], BF16)
            wqup = A.alloc("wqup", [128, 3, 768], BF16)
            wkvup = A.alloc("wkvup", [128, 1024], BF16)
            raw3 = A.alloc("raw3", [128, 3, 512], F32)
            P.dma("pool", wqup[:], mla_w_qup[l].rearrange("(c p) n -> p c n", p=128), writes=["wqup"])
            P.dma("pool", wkvup[:], mla_w_kvup[l], writes=["wkvup"])
            for c in range(3):
                load_gcol(4 + c, mla_g_q[l:l + 1, c * 128:(c + 1) * 128], 128)
            load_gcol(7, mla_g_kv[l:l + 1, :], 128)
            wt, wk, offs = load_w([(OFF["mla_cq"], 384)])
            for (t0, n) in q_blocks:
                for c in range(3):
                    j = proj_fm(wt, wk, offs[0] + c * 128, 128, t0, n)
                    P.op("act", lambda e: e.copy(out=raw3[:, c, 0:n], in_=PB(j, 128, n)), reads=[("pb", j)], writes=[("raw3", c)])
                rs, rsk = mean_rstd(lambda c: raw3[:, c, 0:n], [("raw3", c) for c in range(3)], 128, n, nchunks=3, denom=384.0)
                for c in range(3):
                    P.op("dve", lambda e: e.scalar_tensor_tensor(out=cqn[:, c, t0:t0 + n], in0=raw3[:, c, 0:n], scalar=gcol[:, 4 + c:5 + c], in1=rs[:, 0:n],
                                                                  op0=ALU.mult, op1=ALU.mult), reads=[("raw3", c), rsk, ("gcol", 4 + c)], writes=["cqn"])
            wt, wk, offs = load_w([(OFF["mla_ckv"], 128), (OFF["mla_kpe"], 64)])
            for (t0, n) in all_blocks:
                j = proj_fm(wt, wk, offs[0], 128, t0, n)
                raw, rk = f32r.next()
                P.op("act", lambda e: e.copy(out=raw[:, 0:n], in_=PB(j, 128, n)), reads=[("pb", j)], writes=[rk])
                rs, rsk = mean_rstd(lambda c: raw[:, 0:n], [rk], 128, n)
                P.op("dve", lambda e: e.scalar_tensor_tensor(out=ckvn[:, t0:t0 + n], in0=raw[:, 0:n], scalar=gcol[:, 7:8], in1=rs[:, 0:n],
                                                              op0=ALU.mult, op1=ALU.mult), reads=[rk, rsk, ("gcol", 7)], writes=["ckvn"])
                j = proj_fm(wt, wk, offs[1], 64, t0, n)
                if is_lat(t0):
                    z, zk = f32r.next()
                    P.op("act", lambda e: e.copy(out=z[0:64, 0:n], in_=PB(j, 64, n)), reads=[("pb", j)], writes=[zk])
                    rope_apply(z, zk, 64, t0, n, 64, p16, "perm16", kpe[:, t0:t0 + n], "kpe")
                else:
                    P.op("act", lambda e: e.copy(out=kpe[:, t0:t0 + n], in_=PB(j, 64, n)), reads=[("pb", j)], writes=["kpe"])
            for h in range(4):
                P.set_phase(f"L{l}.mla.proj")
                q_t, qk = qT.next()
                k_t, kk = kT.next()
                v_t, vk = vv.next()
                qp_t, qpk = qpe.next()
                for (t0, n) in all_blocks:
                    j = next_pb()
                    P.op("pe", lambda e: e.matmul(PB(j, 128, n), lhsT=wkvup[:, h * 256:h * 256 + 128], rhs=ckvn[:, t0:t0 + n], start=True, stop=True),
                         reads=["wkvup", "ckvn"], writes=[("pb", j)])
                    P.op("act", lambda e: e.copy(out=k_t[:, t0:t0 + n], in_=PB(j, 128, n)), reads=[("pb", j)], writes=[kk])
                for t in range(NT):
                    j = next_pb()
                    P.op("pe", lambda e: e.matmul(PB(j, 128, 128), lhsT=ckvn[:, t * 128:(t + 1) * 128], rhs=wkvup[:, h * 256 + 128:h * 256 + 256], start=True, stop=True),
                         reads=["wkvup", "ckvn"], writes=[("pb", j)])
                    P.op("act", lambda e: e.copy(out=v_t[:, t, :], in_=PB(j, 128, 128)), reads=[("pb", j)], writes=[(vk, t)])
                for (t0, n) in q_blocks:
                    j = next_pb()
                    for c in range(3):
                        P.op("pe", lambda e: e.matmul(PB(j, 128, n), lhsT=wqup[:, c, h * 192:h * 192 + 128], rhs=cqn[:, c, t0:t0 + n], start=(c == 0), stop=(c == 2)),
                             reads=["wqup", "cqn"], writes=[("pb", j)])
                    P.op("act", lambda e: e.copy(out=q_t[:, t0:t0 + n], in_=PB(j, 128, n)), reads=[("pb", j)], writes=[qk])
                    j = next_pb()
                    for c in range(3):
                        P.op("pe", lambda e: e.matmul(PB(j, 64, n), lhsT=wqup[:, c, h * 192 + 128:h * 192 + 192], rhs=cqn[:, c, t0:t0 + n], start=(c == 0), stop=(c == 2)),
                             reads=["wqup", "cqn"], writes=[("pb", j)])
                    if is_lat(t0):
                        z, zk = f32r.next()
                        P.op("act", lambda e: e.copy(out=z[0:64, 0:n], in_=PB(j, 64, n)), reads=[("pb", j)], writes=[zk])
                        rope_apply(z, zk, 64, t0, n, 64, p16, "perm16", qp_t[:, t0:t0 + n], qpk)
                    else:
                        P.op("act", lambda e: e.copy(out=qp_t[:, t0:t0 + n], in_=PB(j, 64, n)), reads=[("pb", j)], writes=[qpk])
                P.set_phase(f"L{l}.mla.att")
                qp = [(q_t, whole(qk), 0, 128), (qp_t, whole(qpk), 0, 64)]
                kp = [(k_t, whole(kk), 0, 128), (kpe, whole("kpe"), 0, 64)]
                for (q0, nq) in q_blocks:
                    kts = list(range(NT)) if is_lat(q0) else [0, 1]
                    o, ok = attend(qp, kp, v_t, vk, q0, nq, kts, MLA_SCALE)
                    emit_o(4 + h, o[:, 0:nq], [ok], q0, nq)
            P.barrier()
            A.off = b0

        mixer_na()
        b1 = A.off
        p16 = load_rope(64)
        mixer_mla(p16, None)
        mixer_diff(p16, None)
        P.barrier()
        A.off = b1
        p32 = load_rope(128)
        mixer_gqa(None, p32)
        P.barrier()
        A.off = lay_base
        if dbg == f"o{l}":
            d = dbg_tensor("o", [16, 128, NTOK], BF16)
            ot_ = A.alloc("dbgo", [128, NTOK], BF16)
            for c in range(16):
                P.dma("sp", ot_[:], o_scr[c], writes=["dbgo"])
                P.dma("sp", d[c], ot_[:], reads=["dbgo"], writes=["dbg"])
            raise _Stop()

        P.set_phase(f"L{l}.D")
        tiles = list(range(2, NT)) if last else list(range(NT))
        base = A.off
        u2st = Ring(A, "u2st", 2, [128, 16, 128], BF16)
        wo = A.alloc("wo", [128, 16, D], BF16)
        wosrc = w_out[l].rearrange("(k p) n -> p k n", p=128)
        for c4 in range(4):
            P.dma("pool", wo[:, 4 * c4:4 * c4 + 4, :], wosrc[:, 4 * c4:4 * c4 + 4, :], writes=["wo"])
        nb = make_norm_bufs(2)
        gf = A.alloc("gf", [128, D], F32)
        bcast_row(gf[:], g_ffn[l:l + 1, :], "gf")
        mtd = {}
        for slot in (2, 3, 4):
            mtd[slot] = (A.alloc(f"mtd{slot}", [128, D], F32), ("mtd", slot))

        def load_mtd(who):
            for slot in (2, 3, 4):
                bcast_row(mtd[slot][0][:], modv[l, who:who + 1, slot * D:(slot + 1) * D], mtd[slot][1])
            sc, sck = mtd[4]
            P.op("dve", lambda e: e.scalar_tensor_tensor(out=sc[:], in0=sc[:], scalar=1.0, in1=gf[:], op0=ALU.add, op1=ALU.mult),
                 reads=[sck, "gf"], writes=[sck])

        oTr = Ring(A, "oTr", 2, [128, 16, 128], BF16)
        hm = Ring(A, "hm", 2, [128, D], F32)
        if last:
            wr32 = A.alloc("wr32", [128, 16, NEXP], F32)
            br32 = A.alloc("br32", [1, NEXP], F32)
            f32T = Ring(A, "f32T", 2, [128, 16, 128], F32)
            P.dma("sp", wr32[:], moe_wr[0].rearrange("(k p) n -> p k n", p=128), writes=["wr32"])
            P.dma("sp", br32[:], moe_br[0:1, :], writes=["br32"])
        rt = Ring(A, "rt", 2, [128, 4 * NEXP + 8], F32) if last else None
        mt = {(w, s_): mtd[s_] for w in (0, 1) for s_ in (2, 3, 4)}
        cur_who = [None]
        pending = [None]
        for t in tiles:
            who = 1 if t < 2 else 0
            if cur_who[0] != who:
                load_mtd(who)
                cur_who[0] = who
            u2T, u2k = u2st.next()
            ot, otk = oTr.next()
            P.dma("sp", ot[:], o_scr[:, :, t * 128:(t + 1) * 128].rearrange("c p n -> p c n"),
                  reads=[("o", c, q0) for c in range(16) for (q0, nq) in q_blocks if q0 <= t * 128 < q0 + nq], writes=[otk])
            for jb in range(4):
                for c in range(16):
                    P.op("pe", lambda e: e.matmul(PB(jb), lhsT=ot[:, c, :], rhs=wo[:, c, jb * 512:(jb + 1) * 512], start=(c == 0), stop=(c == 15)),
                         reads=[otk, "wo"], writes=[("pb", jb)])
            xt, xk = nb["xt"].next()
            src, sk = h_src(l, t)
            P.dma("sp", xt[:], src, reads=[sk] if sk else [], writes=[xk])
            hmt, hmk = hm.next()
            gt, gtk = mt[(who, 2)]
            P.op("dve", lambda e: e.tensor_tensor(out=hmt[:], in0=pb[:], in1=gt[:], op=ALU.mult), reads=[("pb", j) for j in range(4)] + [gtk], writes=[hmk])
            P.op("pool", lambda e: e.tensor_tensor(out=hmt[:], in0=hmt[:], in1=xt[:], op=ALU.add), reads=[hmk, xk], writes=[hmk])
            P.dma("sp", h_scr[t * 128:(t + 1) * 128, :], hmt[:], reads=[hmk], writes=[("h", t)])
            ft, ftk = f32T.next() if last else (None, None)
            partB = norm_core(nb, hmt, hmk, mt[(who, 4)][0], mt[(who, 4)][1], mt[(who, 3)][0], mt[(who, 3)][1], u2T, u2k, 0,
                              want_f32T=((ft, ftk) if last else None), defer=True)

            def tail(t=t, u2T=u2T, u2k=u2k, ft=ft, ftk=ftk, partB=partB):
                partB()
                P.dma("sp", u2_scr[:, :, t * 128:(t + 1) * 128].rearrange("k p n -> p k n"), u2T[:], reads=[u2k], writes=[("u2", t)])
                if not last:
                    return
                for k in range(16):
                    P.op("pe", lambda e: e.matmul(PA(0, 128, NEXP), lhsT=ft[:, k, :], rhs=wr32[:, k, :], start=(k == 0), stop=False),
                         reads=[ftk, "wr32"], writes=[("pa", 0)])
                P.op("pe", lambda e: e.matmul(PA(0, 128, NEXP), lhsT=ones1[0:1, :], rhs=br32[:], start=False, stop=True),
                     reads=["ones1", "br32"], writes=[("pa", 0)])
                r, rk = rt.next()
                lg, m1, mk1, l2, m2, mk2, ee = r[:, 0:8], r[:, 32:33], r[:, 8:16], r[:, 16:24], r[:, 33:34], r[:, 24:32], r[:, 34:35]
                dd, rr, g2 = r[:, 35:36], r[:, 36:37], r[:, 37:38]
                ti = t - 2
                V = lambda f: P.op("dve", f, reads=[rk], writes=[rk])
                P.op("dve", lambda e: e.tensor_copy(out=lg, in_=PA(0, 128, NEXP)), reads=[("pa", 0)], writes=[rk])
                V(lambda e: e.reduce_max(out=m1, in_=lg, axis=mybir.AxisListType.X))
                V(lambda e: e.tensor_scalar(out=mk1, in0=lg, scalar1=m1, scalar2=None, op0=ALU.is_equal))
                V(lambda e: e.scalar_tensor_tensor(out=l2, in0=mk1, scalar=-1e30, in1=lg, op0=ALU.mult, op1=ALU.add))
                V(lambda e: e.reduce_max(out=m2, in_=l2, axis=mybir.AxisListType.X))
                V(lambda e: e.tensor_scalar(out=mk2, in0=l2, scalar1=m2, scalar2=None, op0=ALU.is_equal))
                V(lambda e: e.tensor_tensor(out=dd, in0=m2, in1=m1, op=ALU.subtract))
                P.op("act", lambda e: e.activation(out=ee, in_=dd, func=AF.Exp), reads=[rk], writes=[rk])
                V(lambda e: e.tensor_scalar(out=rr, in0=ee, scalar1=1.0, scalar2=None, op0=ALU.add))
                V(lambda e: e.reciprocal(out=rr, in_=rr))
                V(lambda e: e.tensor_tensor(out=g2, in0=ee, in1=rr, op=ALU.mult))
                V(lambda e: e.tensor_scalar(out=mk1, in0=mk1, scalar1=rr, scalar2=None, op0=ALU.mult))
                P.op("dve", lambda e: e.scalar_tensor_tensor(out=comb[:, ti, :], in0=mk2, scalar=g2, in1=mk1, op0=ALU.mult, op1=ALU.add),
                     reads=[rk], writes=[("comb", ti)])

            if pending[0] is not None:
                pending[0]()
            pending[0] = tail
        if pending[0] is not None:
            pending[0]()
        P.barrier()
        if dbg == f"h{l}":
            d = dbg_tensor("h", [NTOK, D], F32)
            for t in tiles:
                xt, xk = nb["xt"].next()
                P.dma("sp", xt[:], h_scr[t * 128:(t + 1) * 128, :], writes=[xk])
                P.dma("sp", d[t * 128:(t + 1) * 128, :], xt[:], reads=[xk], writes=["dbg"])
            raise _Stop()
        if dbg == f"u2T{l}":
            d = dbg_tensor("u2T", [16, 128, NTOK], BF16)
            P.dma("sp", d, u2_scr, writes=["dbg"])
            if last:
                d2 = dbg_tensor("comb", [128, 16, NEXP], F32)
                P.dma("sp", d2, comb[:], writes=["dbg2"])
            raise _Stop()
        P.set_phase(f"L{l}.ffn")
        P.relaxed = False
        comb_keep = comb
        A.off = base
        mt = {}
        for who in ((0,) if last else (0, 1)):
            tt_ = A.alloc(f"gtf{who}", [128, D], F32)
            bcast_row(tt_[:], modv[l, who:who + 1, 5 * D:6 * D], ("gtf", who))
            mt[(who, 5)] = (tt_, ("gtf", who))
        gfin = None
        if last:
            gfin = A.alloc("gfin", [128, D], F32)
            bcast_row(gfin[:], g_final[0:1, :], "gfin")
        G = 2
        wgu = Ring(A, "wgu", 2, [128, 2, 16, G * 128], BF16)
        wdr = Ring(A, "wdr", 2, [128, G, D], BF16)
        if last:
            passes = [(CTX, 1024), (CTX + 1024, 1024)]
            experts = [(moe_wg[0, e], moe_wu[0, e], moe_wd[0, e], e) for e in range(NEXP)]
            F = DFFE
        else:
            passes = [(0, 768), (768, 768), (1536, 768)]
            experts = [(ffn_wg[0], ffn_wu[0], ffn_wd[0], None)]
            F = DFF
        TBMAX = max(n for _, n in passes)
        y = A.alloc("y", [128, TBMAX // 128, D], F32)
        Hr = Ring(A, "H", 2, [128, G, TBMAX], BF16)
        sr = Ring(A, "s", 3, [128, 512], F32)
        fjunk = A.alloc("fjunk", [128, D], BF16) if last else None
        fss = Ring(A, "fss", 2, [128, 2], F32) if last else None
        hx = Ring(A, "hx", 1 if last else 2, [128, D], F32)
        u2pr = Ring(A, "u2p", 1, [128, 16, TBMAX], BF16)
        pdi = [0]
        for (p0, pn) in passes:
            ntile = pn // 128
            u2T, u2pk = u2pr.next()
            P.dma("sp", u2T[:, :, 0:pn], u2_scr[:, :, p0:p0 + pn].rearrange("k p n -> p k n"), writes=[u2pk])
            tbs = [(i, min(512, pn - i)) for i in range(0, pn, 512)]
            first_write = [True] * (ntile * 4)
            gi = [0]
            groups = []
            for (wg_, wu_, wd_, eidx) in experts:
                wgs = wg_.rearrange("(k p) n -> p k n", p=128)
                wus = wu_.rearrange("(k p) n -> p k n", p=128)
                wds = wd_.rearrange("(f p) n -> p f n", p=128)
                for fg in range(F // (G * 128)):
                    groups.append((wgs, wus, wds, eidx, fg))
            units = [(tb0, tbn, fc) for (tb0, tbn) in tbs for fc in range(G)]
            dtiles = [(tt, jb) for tt in range(ntile) for jb in range(4)]
            nd = len(dtiles)
            nu = len(units)

            def gate_up(wt, wk, Ht, Hk, unit, prev=None, lo=0, hi=0):
                tb0, tbn, fc = unit
                par = gi[0] % 2
                gi[0] += 1
                nmm = 0
                dpos = lo
                ndn = hi - lo
                for w in range(2):
                    bank = 2 * par + w
                    for k in range(16):
                        P.op("pe", lambda e: e.matmul(PA(bank, 128, tbn), lhsT=wt[:, w, k, fc * 128:(fc + 1) * 128],
                                                      rhs=u2T[:, k, tb0:tb0 + tbn], start=(k == 0), stop=(k == 15)),
                             reads=[wk, u2pk], writes=[("pa", bank)])
                        nmm += 1
                        if prev is not None and ndn > 0:
                            want = lo + (ndn * nmm) // 32
                            if want > dpos:
                                down(prev, dpos, want)
                                dpos = want
                st, stk = sr.next()
                P.op("act", lambda e: e.activation(out=st[:, 0:tbn], in_=PA(2 * par, 128, tbn), func=AF.Silu), reads=[("pa", 2 * par)], writes=[stk])
                P.op("dve", lambda e: e.tensor_tensor(out=Ht[:, fc, tb0:tb0 + tbn], in0=st[:, 0:tbn], in1=PA(2 * par + 1, 128, tbn), op=ALU.mult),
                     reads=[stk, ("pa", 2 * par + 1)], writes=[(Hk, fc, tb0)])

            def down(prev, lo, hi):
                Ht, Hk, wdt, wdk, eidx = prev
                for (tt, jb) in dtiles[lo:hi]:
                    tb0 = (tt * 128 // 512) * 512
                    j = pdi[0] % 4
                    pdi[0] += 1
                    for fc in range(G):
                        P.op("pe", lambda e: e.matmul(PB(j), lhsT=Ht[:, fc, tt * 128:(tt + 1) * 128], rhs=wdt[:, fc, jb * 512:(jb + 1) * 512],
                                                      start=(fc == 0), stop=(fc == G - 1)),
                             reads=[(Hk, fc, tb0), wdk], writes=[("pb", j)])
                    ysl = y[:, tt, jb * 512:(jb + 1) * 512]
                    yk = ("y", tt, jb)
                    fw = first_write[tt * 4 + jb]
                    first_write[tt * 4 + jb] = False
                    if eidx is None:
                        if fw:
                            P.op("dve", lambda e: e.tensor_copy(out=ysl, in_=PB(j)), reads=[("pb", j)], writes=[yk])
                        else:
                            P.op("dve", lambda e: e.tensor_tensor(out=ysl, in0=PB(j), in1=ysl, op=ALU.add), reads=[("pb", j), yk], writes=[yk])
                    else:
                        ccol = comb_keep[:, (p0 - CTX) // 128 + tt, eidx:eidx + 1]
                        if fw:
                            P.op("dve", lambda e: e.tensor_scalar(out=ysl, in0=PB(j), scalar1=ccol, scalar2=None, op0=ALU.mult),
                                 reads=[("pb", j), "comb_keep"], writes=[yk])
                        else:
                            P.op("dve", lambda e: e.scalar_tensor_tensor(out=ysl, in0=PB(j), scalar=ccol, in1=ysl, op0=ALU.mult, op1=ALU.add),
                                 reads=[("pb", j), yk, "comb_keep"], writes=[yk])

            prev = None
            for (wgs, wus, wds, eidx, fg) in groups:
                wt, wk = wgu.next()
                P.dma("pool", wt[:, 0], wgs[:, :, fg * G * 128:(fg + 1) * G * 128], writes=[wk])
                P.dma("pool", wt[:, 1], wus[:, :, fg * G * 128:(fg + 1) * G * 128], writes=[wk])
                wdt, wdk = wdr.next()
                P.dma("pool", wdt[:], wds[:, fg * G:(fg + 1) * G, :], writes=[wdk])
                Ht, Hk = Hr.next()
                for ui, unit in enumerate(units):
                    gate_up(wt, wk, Ht, Hk, unit)
                    if prev is not None:
                        down(prev, nd * ui // nu, nd * (ui + 1) // nu)
                prev = (Ht, Hk, wdt, wdk, eidx)
            down(prev, 0, nd)
            for tt in range(ntile):
                t = (p0 // 128) + tt
                who = 1 if t < 2 else 0
                ht, hk = hx.next()
                P.dma("sp", ht[:], h_scr[t * 128:(t + 1) * 128, :], reads=[("h", t)], writes=[hk])
                gt, gtk = mt[(who, 5)]
                yk4 = [("y", tt, jb) for jb in range(4)]
                P.op("dve", lambda e: e.tensor_tensor(out=y[:, tt, :], in0=y[:, tt, :], in1=gt[:], op=ALU.mult), reads=yk4 + [gtk], writes=yk4)
                P.op("dve", lambda e: e.tensor_tensor(out=ht[:], in0=ht[:], in1=y[:, tt, :], op=ALU.add), reads=yk4 + [hk], writes=[hk])
                if not last:
                    P.dma("sp", h_scr[t * 128:(t + 1) * 128, :], ht[:], reads=[hk], writes=[("h", t)])
                else:
                    sst, ssk = fss.next()
                    P.op("dve", lambda e: e.scalar_tensor_tensor(out=fjunk[:], in0=ht[:], scalar=1.0, in1=ht[:], op0=ALU.mult, op1=ALU.mult,
                                                                  accum_out=sst[:, 0:1]), reads=[hk], writes=["fjunk", ssk])
                    P.op("act", lambda e: e.activation(out=sst[:, 1:2], in_=sst[:, 0:1], func=AF.Sqrt, bias=epsb[:, 0:1], scale=1.0 / D),
                         reads=[ssk, "epsb"], writes=[ssk])
                    P.op("dve", lambda e: e.reciprocal(out=sst[:, 1:2], in_=sst[:, 1:2]), reads=[ssk], writes=[ssk])
                    P.op("dve", lambda e: e.scalar_tensor_tensor(out=y[:, tt, :], in0=ht[:], scalar=sst[:, 1:2], in1=gfin[:], op0=ALU.mult, op1=ALU.mult),
                         reads=[hk, ssk, "gfin"] + yk4, writes=yk4)
                    P.dma("sp", out[(t - 2) * 128:(t - 1) * 128, :], y[:, tt, :], reads=yk4, writes=[("out", t)])
        P.barrier()
        P.relaxed = True
        A.off = lay_base

    phase_mod()
    try:
        for l in range(2):
            layer(l)
            if dbg == f"hend{l}":
                d = dbg_tensor("hend", [NTOK, D], F32)
                P.dma("sp", d, h_scr, writes=["dbg"])
                raise _Stop()
    except _Stop:
        pass
    P.barrier()
    P.set_phase("end")
    build_program.phase_log = P.phase_log
    return nc, dbg_out


def _consts():
    ident = np.eye(128, dtype=np.float32)
    idx = np.arange(128)
    perm16 = np.zeros((128, 128), np.float32)
    perm16[idx ^ 16, idx] = 1.0
    perm32 = np.zeros((128, 128), np.float32)
    perm32[idx ^ 32, idx] = 1.0
    n = np.arange(SEQ)
    row = (n // GRID_W).astype(np.float32)
    col = (n % GRID_W).astype(np.float32)

    def tables(dim):
        half = dim // 2
        q = dim // 4
        freqs = (10000.0 ** (-np.arange(0, half, 2, dtype=np.float32) / half)).astype(np.float32)
        C = np.zeros((dim, SEQ), np.float32)
        S = np.zeros((dim, SEQ), np.float32)
        for i, pos in enumerate((row, col)):
            ang = (pos[None, :] * freqs[:, None]).astype(np.float32)
            for j in range(2):
                p0 = i * 2 * q + j * q
                C[p0:p0 + q] = np.cos(ang)
                S[p0:p0 + q] = np.sin(ang) * (-1.0 if j == 0 else 1.0)
        return C, S

    C128, S128 = tables(128)
    C64, S64 = tables(64)
    rope = np.stack([C128, S128, np.concatenate([C64, C64]), np.concatenate([S64, S64])]).astype(np.float32)
    kc = np.arange(64)[:, None]
    qc = np.arange(64)[None, :]
    cs = np.clip(qc - 8, 0, 64 - 16)
    cm = ((kc >= cs) & (kc < cs + 16)).astype(np.float32)
    cmask = np.concatenate([cm, cm], 0)
    return dict(c_ident=ident, c_perm16=perm16, c_perm32=perm32, c_rope=rope, c_cmask=cmask)


_CACHE = {}


def kernel(**inputs):
    dbg = inputs.pop("_dbg", None)
    ncores = inputs.pop("_ncores", 8)
    f = lambda k: np.ascontiguousarray(np.asarray(inputs[k], dtype=np.float32))
    key = ("prog", dbg)
    if key not in _CACHE:
        _CACHE[key] = build_program(dbg)
    nc, dbg_out = _CACHE[key]
    shared = {k: f(k) for k in ("w_mod", "b_mod", "g_mix", "w_in", "mla_g_q", "mla_w_qup", "mla_g_kv", "mla_w_kvup",
                                "gqa_g_q", "gqa_g_k", "diff_g_sub", "w_out", "g_ffn", "ffn_w_gate", "ffn_w_up",
                                "ffn_w_down", "moe_w_router", "moe_b_router", "moe_w_gate", "moe_w_up", "moe_w_down")}
    shared["g_final"] = f("g_final").reshape(1, D)
    shared["c_ctx"] = f("c_ctx").reshape(1, D)
    shared["diff_lam"] = np.ascontiguousarray(np.stack([f("diff_lq1"), f("diff_lk1"), f("diff_lq2"), f("diff_lk2")], axis=1))
    kc = np.arange(64)[:, None]
    qc = np.arange(64)[None, :]
    col_off = np.clip(kc - qc + 15, 0, 30)
    shared["na_bias_exp"] = np.ascontiguousarray(f("na_rel_bias")[:, :, :, col_off])
    shared.update(_consts())
    xs, cs, ctxs = f("x"), f("c"), f("ctx")
    in_maps = []
    for b in range(ncores):
        m = dict(shared)
        m["x"] = xs[b]
        m["c"] = cs[b:b + 1]
        m["ctx"] = ctxs[b]
        in_maps.append(m)
    res = run_bass_kernel_spmd(nc, in_maps, core_ids=list(range(ncores)))
    if dbg is not None:
        return res.results
    return np.stack([np.asarray(r["out"], dtype=np.float32) for r in res.results], axis=0)
```

```python
import math
import numpy as np
import concourse.bass as bass
import concourse.mybir as mybir
from concourse.bass_utils import run_bass_kernel_spmd

F32 = mybir.dt.float32
BF16 = mybir.dt.bfloat16
AF = mybir.ActivationFunctionType
ALU = mybir.AluOpType

D = 2048
SEQ = 2048
CTX = 256
NTOK = SEQ + CTX
NT = NTOK // 128
KC = D // 128
EPS = 1e-6
GRID_W = 64
DFF = 5632
NEXP = 8
DFFE = 7168
IN_COLS = 4672
OFF = dict(na_q=0, na_k=512, na_v=1024, mla_cq=1536, mla_ckv=1920, mla_kpe=2048, gqa_q=2112, gqa_k=2624,
           gqa_v=2880, diff_q=3136, diff_k=3648, diff_v=4160)
NA_SCALE = 128 ** -0.5
MLA_SCALE = 192 ** -0.5
GQA_SCALE = 128 ** -0.5
DIFF_SCALE = 64 ** -0.5


class Prog:
    NDSEM = 12

    def __init__(self, nc, same_engine_sync=True):
        self.nc = nc
        self.eng = {"pe": nc.tensor, "act": nc.scalar, "dve": nc.vector, "pool": nc.gpsimd, "sp": nc.sync}
        self.csem = {e: nc.alloc_semaphore(name=f"cs_{e}") for e in ("pe", "act", "dve", "pool")}
        self.ccnt = {e: 0 for e in self.csem}
        self.dsem = {q: [nc.alloc_semaphore(name=f"ds_{q}{i}") for i in range(self.NDSEM)]
                     for q in ("sp", "pool")}
        self.dcnt = {q: 0 for q in self.dsem}
        self.waited = {}
        self.res = {}
        self.same_engine_sync = same_engine_sync
        self.n_inst = 0
        self.phase = "init"
        self.phase_log = []

    def set_phase(self, name):
        self.phase = name
        self.phase_log.append((name, self.ccnt["pe"]))

    def _sem(self, sk):
        return self.csem[sk[1]] if sk[0] == "c" else self.dsem[sk[1]][sk[2]]

    def _wait(self, e, tok, raw=True):
        sk, v = tok
        if sk[0] == "c" and sk[1] == e and (e == "pe" or not self.same_engine_sync or not raw):
            return
        key = (e, sk)
        if self.waited.get(key, 0) >= v:
            return
        self.waited[key] = v
        self.eng[e].wait_ge(self._sem(sk), v)

    def _deps(self, reads, writes):
        deps = []
        for r in reads:
            st = self.res.get(r)
            if st and st[0] is not None:
                deps.append((st[0], True))
        for w in writes:
            st = self.res.get(w)
            if st:
                if st[0] is not None:
                    deps.append((st[0], False))
                deps.extend((it, False) for it in st[1].items())
        return deps

    def _record(self, tok, reads, writes):
        sk, v = tok
        for r in reads:
            st = self.res.setdefault(r, [None, {}])
            if st[1].get(sk, 0) < v:
                st[1][sk] = v
        for w in writes:
            self.res[w] = [tok, {}]

    def op(self, e, fn, reads=(), writes=()):
        for d, raw in self._deps(reads, writes):
            self._wait(e, d, raw)
        inst = fn(self.eng[e])
        inst.then_inc(self.csem[e], 1)
        self.ccnt[e] += 1
        self.n_inst += 1
        tok = (("c", e), self.ccnt[e])
        self._record(tok, reads, writes)
        return tok

    def dma(self, q, out, in_, reads=(), writes=(), **kw):
        i = self.dcnt[q]
        slot, rnd = i % self.NDSEM, i // self.NDSEM
        for d, raw in self._deps(reads, writes):
            self._wait(q, d, True)
        if rnd > 0:
            self._wait(q, (("d", q, slot), 16 * rnd))
        inst = self.eng[q].dma_start(out=out, in_=in_, **kw)
        inst.then_inc(self.dsem[q][slot], 16)
        self.dcnt[q] += 1
        self.n_inst += 1
        tok = (("d", q, slot), 16 * (rnd + 1))
        self._record(tok, reads, writes)
        return tok

    def barrier(self, engines=("pe", "act", "dve", "pool", "sp")):
        toks = [(("c", x), n) for x, n in self.ccnt.items() if n > 0]
        for q, n in self.dcnt.items():
            for s in range(min(n, self.NDSEM)):
                toks.append((("d", q, s), 16 * ((n - 1 - s) // self.NDSEM + 1)))
        for e in engines:
            for t in toks:
                sk, v = t
                if sk[0] == "c" and sk[1] == e:
                    continue
                key = (e, sk)
                if self.waited.get(key, 0) >= v:
                    continue
                self.waited[key] = v
                self.eng[e].wait_ge(self._sem(sk), v)
        if len(engines) == 5:
            self.res = {}


class _Stop(Exception):
    pass


class Arena:
    def __init__(self, nc, limit):
        self.nc, self.off, self.limit, self.n = nc, 18560, limit, 0

    def alloc(self, name, shape, dt):
        nbytes = int(np.prod(shape[1:])) * (2 if dt == BF16 else 4)
        nbytes = (nbytes + 63) // 64 * 64
        assert self.off + nbytes <= self.limit, (name, self.off, nbytes)
        self.n += 1
        t = self.nc.alloc_sbuf_tensor_at(f"{name}_{self.n}", list(shape), dt, offset=self.off)
        self.off += nbytes
        return t


class Ring:
    def __init__(self, A, name, n, shape, dt):
        self.name = name
        self.t = [A.alloc(f"{name}{i}", shape, dt) for i in range(n)]
        self.i = 0

    def next(self):
        k = self.i % len(self.t)
        self.i += 1
        return self.t[k], (self.name, k)


def build_program(dbg=None):
    nc = bass.Bass("TRN2", target_bir_lowering=False)
    P = Prog(nc)
    din = lambda n, s, dt=F32: nc.dram_tensor(n, list(s), dt, kind="ExternalInput")
    x = din("x", [SEQ, D]).ap()
    ctx = din("ctx", [CTX, D]).ap()
    c_in = din("c", [1, D]).ap()
    cc_in = din("c_ctx", [1, D]).ap()
    w_mod = din("w_mod", [2, D, 6 * D]).ap()
    b_mod = din("b_mod", [2, 6 * D]).ap()
    g_mix = din("g_mix", [2, D]).ap()
    w_in = din("w_in", [2, D, IN_COLS]).ap()
    na_bias = din("na_bias_exp", [2, 4, 15, 64, 64]).ap()
    mla_g_q = din("mla_g_q", [2, 384]).ap()
    mla_w_qup = din("mla_w_qup", [2, 384, 768]).ap()
    mla_g_kv = din("mla_g_kv", [2, 128]).ap()
    mla_w_kvup = din("mla_w_kvup", [2, 128, 1024]).ap()
    gqa_g_q = din("gqa_g_q", [2, 128]).ap()
    gqa_g_k = din("gqa_g_k", [2, 128]).ap()
    dlam = din("diff_lam", [2, 4, 64]).ap()
    diff_g_sub = din("diff_g_sub", [2, 128]).ap()
    w_out = din("w_out", [2, D, D]).ap()
    g_ffn = din("g_ffn", [2, D]).ap()
    ffn_wg = din("ffn_w_gate", [1, D, DFF]).ap()
    ffn_wu = din("ffn_w_up", [1, D, DFF]).ap()
    ffn_wd = din("ffn_w_down", [1, DFF, D]).ap()
    moe_wr = din("moe_w_router", [1, D, NEXP]).ap()
    moe_br = din("moe_b_router", [1, NEXP]).ap()
    moe_wg = din("moe_w_gate", [1, NEXP, D, DFFE]).ap()
    moe_wu = din("moe_w_up", [1, NEXP, D, DFFE]).ap()
    moe_wd = din("moe_w_down", [1, NEXP, DFFE, D]).ap()
    g_final = din("g_final", [1, D]).ap()
    c_ident = din("c_ident", [128, 128]).ap()
    c_perm16 = din("c_perm16", [128, 128]).ap()
    c_perm32 = din("c_perm32", [128, 128]).ap()
    c_rope = din("c_rope", [4, 128, SEQ]).ap()
    c_cmask = din("c_cmask", [128, 64]).ap()
    out = nc.dram_tensor("out", [SEQ, D], F32, kind="ExternalOutput").ap()
    dbg_out = {}

    def dbg_tensor(name, shape, dt=F32):
        t = nc.dram_tensor("dbg_" + name, list(shape), dt, kind="ExternalOutput").ap()
        dbg_out[name] = t
        return t

    h_scr = nc.dram_tensor("h_scr", [NTOK, D], F32, kind="Internal").ap()
    modv = nc.dram_tensor("modv", [2, 2, 6 * D], F32, kind="Internal").ap()
    o_scr = nc.dram_tensor("o_scr", [16, 128, NTOK], BF16, kind="Internal").ap()
    u2_scr = nc.dram_tensor("u2_scr", [16, 128, NTOK], BF16, kind="Internal").ap()

    A = Arena(nc, 229000)
    pa = nc.alloc_psum_tensor("pa", [128, 2048], F32)
    pb = nc.alloc_psum_tensor("pb", [128, 2048], F32)
    pa_b = pa[:].bitcast(BF16)
    PA = lambda j, r=128, n=512: pa[0:r, j * 512:j * 512 + n]
    PB = lambda j, r=128, n=512: pb[0:r, j * 512:j * 512 + n]

    identf = A.alloc("identf", [128, 128], F32)
    identb = A.alloc("identb", [128, 128], BF16)
    onesb = A.alloc("onesb", [128, 128], BF16)
    onesf = A.alloc("onesf", [128, 128], F32)
    ones1 = A.alloc("ones1", [128, 128], F32)
    epsb = A.alloc("epsb", [128, 1], F32)
    comb = A.alloc("comb", [128, 16, NEXP], F32)
    P.dma("sp", identf[:], c_ident, writes=["identf"])
    P.op("dve", lambda e: e.tensor_copy(out=identb[:], in_=identf[:]), reads=["identf"], writes=["identb"])
    P.op("dve", lambda e: e.memset(onesb[:], 1.0), writes=["onesb"])
    P.op("dve", lambda e: e.memset(onesf[:], 1.0 / 128), writes=["onesf"])
    P.op("dve", lambda e: e.memset(ones1[:], 1.0), writes=["ones1"])
    P.op("dve", lambda e: e.memset(epsb[:], EPS), writes=["epsb"])
    CONST_END = A.off

    def bcast_row(dst, src_row, key, q="sp"):
        return P.dma(q, dst, src_row.partition_broadcast(128), writes=[key])

    def phase_mod():
        P.set_phase("mod")
        base = A.off
        cT = A.alloc("cT", [128, 16, 2], F32)
        sT = A.alloc("sT", [128, 16, 2], F32)
        wm = Ring(A, "wm", 3, [128, 16, 512], F32)
        bmr = Ring(A, "bmr", 3, [1, 512], F32)
        mo = Ring(A, "mo", 3, [2, 512], F32)
        with nc.allow_non_contiguous_dma(reason="tiny transposed load of c"):
            P.dma("sp", cT[:, :, 0], c_in.rearrange("o (k p) -> p (o k)", p=128), writes=["cT0"])
            P.dma("sp", cT[:, :, 1], cc_in.rearrange("o (k p) -> p (o k)", p=128), writes=["cT1"])
        P.op("act", lambda e: e.activation(out=sT[:], in_=cT[:], func=AF.Silu), reads=["cT0", "cT1"], writes=["sT"])
        for l in range(2):
            wsrc = w_mod[l].rearrange("(k p) n -> p k n", p=128)
            for cb in range(24):
                wt, wk = wm.next()
                P.dma("sp", wt[:], wsrc[:, :, cb * 512:(cb + 1) * 512], writes=[wk])
                bt, bk = bmr.next()
                P.dma("sp", bt[:], b_mod[l:l + 1, cb * 512:(cb + 1) * 512], writes=[bk])
                for k in range(16):
                    P.op("pe", lambda e: e.matmul(PA(cb % 2, 2), lhsT=sT[:, k, :], rhs=wt[:, k, :], start=(k == 0), stop=False),
                         reads=["sT", wk], writes=[("pa", cb % 2)])
                P.op("pe", lambda e: e.matmul(PA(cb % 2, 2), lhsT=ones1[0:1, 0:2], rhs=bt[:], start=False, stop=True),
                     reads=["ones1", bk], writes=[("pa", cb % 2)])
                mt, mk = mo.next()
                P.op("act", lambda e: e.copy(out=mt[:], in_=PA(cb % 2, 2)), reads=[("pa", cb % 2)], writes=[mk])
                P.dma("pool", modv[l, :, cb * 512:(cb + 1) * 512], mt[:], reads=[mk], writes=[("modv", l)])
        P.barrier()
        A.off = base

    def make_norm_bufs(nr=2):
        nb = dict(
            xt=Ring(A, "n_xt", 2, [128, D], F32),
            y1=Ring(A, "n_y1", nr, [128, D], F32),
            ub=Ring(A, "n_ub", nr, [128, D], BF16),
            junk=A.alloc("n_junk", [128, D], BF16),
            ss=Ring(A, "n_ss", 4, [128, 2], F32),
        )
        return nb

    tcount = [0]

    def norm_core(nb, xt, xk, G, Gk, Bt, Bk, dstT, dst_key, col0, want_f32T=None, defer=False):
        sst, ssk = nb["ss"].next()
        P.op("dve", lambda e: e.scalar_tensor_tensor(out=nb["junk"][:], in0=xt[:], scalar=1.0, in1=xt[:], op0=ALU.mult,
                                                      op1=ALU.mult, accum_out=sst[:, 0:1]),
             reads=[xk], writes=["n_junk", ssk])
        P.op("act", lambda e: e.activation(out=sst[:, 1:2], in_=sst[:, 0:1], func=AF.Sqrt, bias=epsb[:, 0:1], scale=1.0 / D),
             reads=[ssk, "epsb"], writes=[ssk])
        P.op("dve", lambda e: e.reciprocal(out=sst[:, 1:2], in_=sst[:, 1:2]), reads=[ssk], writes=[ssk])
        y1, y1k = nb["y1"].next()
        P.op("dve", lambda e: e.scalar_tensor_tensor(out=y1[:], in0=xt[:], scalar=sst[:, 1:2], in1=G[:], op0=ALU.mult, op1=ALU.mult),
             reads=[xk, ssk, Gk], writes=[y1k])
        i = tcount[0] % 2
        tcount[0] += 1
        if want_f32T is None:
            ub, ubk = nb["ub"].next()
            if Bt is not None:
                P.op("pool", lambda e: e.tensor_tensor(out=ub[:], in0=y1[:], in1=Bt[:], op=ALU.add), reads=[y1k, Bk], writes=[ubk])
            else:
                P.op("pool", lambda e: e.tensor_copy(out=ub[:], in_=y1[:]), reads=[y1k], writes=[ubk])
            pt = pa_b[:, i * 2048:(i + 1) * 2048].rearrange("p (c n) -> p c n", c=16)
            pk = [("pa", 2 * i), ("pa", 2 * i + 1)]

            def partB():
                for j in range(16):
                    P.op("pe", lambda e: e.transpose(pt[:, j, :], ub[:, j * 128:(j + 1) * 128], identb[:]),
                         reads=[ubk, "identb"], writes=[pk[j // 8]])
                P.op("act", lambda e: e.copy(out=dstT[:, :, col0:col0 + 128], in_=pt), reads=pk, writes=[dst_key])
        else:
            f32T, f32k = want_f32T
            if Bt is not None:
                P.op("pool", lambda e: e.tensor_tensor(out=y1[:], in0=y1[:], in1=Bt[:], op=ALU.add), reads=[y1k, Bk], writes=[y1k])

            def partB():
                for hh in range(4):
                    for j in range(4):
                        jj = hh * 4 + j
                        P.op("pe", lambda e: e.transpose(PA(hh)[:, j * 128:(j + 1) * 128], y1[:, jj * 128:(jj + 1) * 128], identf[:]),
                             reads=[y1k, "identf"], writes=[("pa", hh)])
                P.op("act", lambda e: e.copy(out=f32T[:].rearrange("p c n -> p (c n)"), in_=pa[:]), reads=[("pa", j) for j in range(4)], writes=[f32k])
                P.op("pool", lambda e: e.tensor_copy(out=dstT[:, :, col0:col0 + 128], in_=f32T[:]), reads=[f32k], writes=[dst_key])

        if defer:
            return partB
        partB()

    def h_src(l, t):
        if l == 0:
            return (ctx[t * 128:(t + 1) * 128, :] if t < 2 else x[(t - 2) * 128:(t - 1) * 128, :]), None
        return h_scr[t * 128:(t + 1) * 128, :], ("h", t)

    def load_mod_tiles(l, names, with_g=None):
        tiles = {}
        for who in (0, 1):
            for (slot, kind) in names:
                t = A.alloc(f"mt{who}{slot}", [128, D], F32)
                key = ("mt", who, slot)
                bcast_row(t[:], modv[l, who:who + 1, slot * D:(slot + 1) * D], key)
                tiles[(who, slot)] = (t, key)
        return tiles

    def layer(l):
        last = l == 1
        lam_init = 0.8 - 0.6 * math.exp(-0.3 * l)
        lay_base = A.off
        uT = A.alloc("uT", [128, 16, NTOK], BF16)

        P.set_phase(f"L{l}.N1")
        base = A.off
        nb = make_norm_bufs()
        gm = A.alloc("gm", [128, D], F32)
        bcast_row(gm[:], g_mix[l:l + 1, :], "gm")
        mt = load_mod_tiles(l, [(0, "sh"), (1, "sc")])
        for who in (0, 1):
            sc, sck = mt[(who, 1)]
            P.op("dve", lambda e: e.scalar_tensor_tensor(out=sc[:], in0=sc[:], scalar=1.0, in1=gm[:], op0=ALU.add, op1=ALU.mult),
                 reads=[sck, "gm"], writes=[sck])
        for t in range(NT):
            who = 1 if t < 2 else 0
            xt, xk = nb["xt"].next()
            src, sk = h_src(l, t)
            P.dma("sp", xt[:], src, reads=[sk] if sk else [], writes=[xk])
            norm_core(nb, xt, xk, mt[(who, 1)][0], mt[(who, 1)][1], mt[(who, 0)][0], mt[(who, 0)][1], uT, ("uT", t), t * 128)
        P.barrier()
        A.off = base
        if dbg == f"uT{l}":
            d = dbg_tensor("uT", [128, 16, NTOK], BF16)
            P.dma("sp", d, uT[:], reads=[("uT", t) for t in range(NT)], writes=["dbg"])
            raise _Stop()

        uT_keys = [("uT", t) for t in range(NT)]
        lat_blocks = [(CTX + 512 * i, 512) for i in range(4)]
        ctx_blocks = [(0, CTX)]
        q_blocks = lat_blocks + ([] if last else ctx_blocks)
        all_blocks = ctx_blocks + lat_blocks
        win = w_in[l].rearrange("(k p) n -> p k n", p=128)

        base = A.off
        wring = Ring(A, "wr", 2, [128, 16, 384], BF16)
        qT = Ring(A, "qT", 2, [128, NTOK], BF16)
        kT = Ring(A, "kT", 2, [128, NTOK], BF16)
        vv = Ring(A, "vv", 2, [128, NT, 128], BF16)
        Er = Ring(A, "E", 3, [128, 2, 512], BF16)
        e0r = Ring(A, "e0r", 2, [128, 2, 128], F32)
        f32r = Ring(A, "f32r", 6, [128, 512], F32)
        ofm = Ring(A, "ofm", 3, [128, 512], BF16)
        gcol = A.alloc("gcol", [128, 8], F32)
        pbi = [0]

        acnt = [0]

        def next_pb():
            j = pbi[0] % 4
            pbi[0] += 1
            return j

        def load_w(cols):
            wt, wk = wring.next()
            o = 0
            offs = []
            for (c0, n) in cols:
                P.dma("pool", wt[:, :, o:o + n], win[:, :, c0:c0 + n], writes=[wk])
                offs.append(o)
                o += n
            return wt, wk, offs

        def proj_fm(wt, wk, o, n, tok0, ntok):
            j = next_pb()
            for k in range(16):
                P.op("pe", lambda e: e.matmul(PB(j, n, ntok), lhsT=wt[:, k, o:o + n], rhs=uT[:, k, tok0:tok0 + ntok],
                                              start=(k == 0), stop=(k == 15)),
                     reads=[wk] + uT_keys[tok0 // 128:(tok0 + ntok) // 128], writes=[("pb", j)])
            return j

        def proj_v(wt, wk, o, vt, vk):
            for t in range(NT):
                j = next_pb()
                for k in range(16):
                    P.op("pe", lambda e: e.matmul(PB(j, 128, 128), lhsT=uT[:, k, t * 128:(t + 1) * 128], rhs=wt[:, k, o:o + 128],
                                                  start=(k == 0), stop=(k == 15)),
                         reads=[wk, ("uT", t)], writes=[("pb", j)])
                P.op("act", lambda e: e.copy(out=vt[:, t, :], in_=PB(j, 128, 128)), reads=[("pb", j)], writes=[(vk, t)])

        def mean_rstd(src_ap, src_keys, n_part, ntok, nchunks=1, denom=128.0):
            sq, sqk = f32r.next()
            rs, rsk = f32r.next()
            j = next_pb()
            for c in range(nchunks):
                s_ap = src_ap(c)
                P.op("dve", lambda e: e.tensor_tensor(out=sq[0:n_part, 0:ntok], in0=s_ap, in1=s_ap, op=ALU.mult), reads=src_keys, writes=[sqk])
                P.op("pe", lambda e: e.matmul(PB(j, 128, ntok), lhsT=ones1[0:n_part, :], rhs=sq[0:n_part, 0:ntok], start=(c == 0), stop=(c == nchunks - 1)),
                     reads=[sqk, "ones1"], writes=[("pb", j)])
            P.op("act", lambda e: e.activation(out=rs[:, 0:ntok], in_=PB(j, 128, ntok), func=AF.Sqrt, bias=epsb[:, 0:1], scale=1.0 / denom),
                 reads=[("pb", j), "epsb"], writes=[rsk])
            P.op("dve", lambda e: e.reciprocal(out=rs[:, 0:ntok], in_=rs[:, 0:ntok]), reads=[rsk], writes=[rsk])
            return rs, rsk

        rope_t = {}

        def rope_apply(z, zk, n_part, tok0, ntok, tab, perm, permk, dst_ap, dst_key):
            C, S = rope_t[tab]
            n0 = tok0 - CTX
            j = next_pb()
            P.op("pe", lambda e: e.matmul(PB(j, n_part, ntok), lhsT=perm[0:n_part, 0:n_part], rhs=z[0:n_part, 0:ntok], start=True, stop=True),
                 reads=[zk, permk], writes=[("pb", j)])
            t1, t1k = f32r.next()
            P.op("pool", lambda e: e.tensor_tensor(out=t1[0:n_part, 0:ntok], in0=z[0:n_part, 0:ntok], in1=C[0:n_part, n0:n0 + ntok], op=ALU.mult),
                 reads=[zk, "rope"], writes=[t1k])
            t2, t2k = f32r.next()
            P.op("dve", lambda e: e.tensor_tensor(out=t2[0:n_part, 0:ntok], in0=PB(j, n_part, ntok), in1=S[0:n_part, n0:n0 + ntok], op=ALU.mult),
                 reads=[("pb", j), "rope"], writes=[t2k])
            P.op("dve", lambda e: e.tensor_tensor(out=dst_ap, in0=t1[0:n_part, 0:ntok], in1=t2[0:n_part, 0:ntok], op=ALU.add),
                 reads=[t1k, t2k], writes=[dst_key])

        def attend(q_parts, k_parts, vt, vk, q0, nq, key_tiles, scale, masks=None):
            nk = len(key_tiles)
            par = acnt[0] % 2
            acnt[0] += 1
            OB = PB(2 * par, 128, nq)
            DB = PB(2 * par + 1, 128, nq)
            obk = ("pb", 2 * par)
            dbk = ("pb", 2 * par + 1)
            pairs = [key_tiles[i:i + 2] for i in range(0, nk, 2)]
            npr = len(pairs)
            pa3 = pa[:].rearrange("p (b n) -> p b n", b=4)

            def emit_S(pi_):
                b0 = 2 * (pi_ % 2)
                for idx, kt in enumerate(pairs[pi_]):
                    for pi, ((qt_, qk_, p0, p1), (kt_, kk_, _, _)) in enumerate(zip(q_parts, k_parts)):
                        P.op("pe", lambda e: e.matmul(PA(b0 + idx, 128, nq), lhsT=kt_[p0:p1, kt * 128:(kt + 1) * 128], rhs=qt_[p0:p1, q0:q0 + nq],
                                                      start=(pi == 0), stop=(pi == len(q_parts) - 1)),
                             reads=[qk_(q0, nq), kk_(kt)], writes=[("pa", b0 + idx)])

            emit_S(0)
            step = 0
            for pi_, pr in enumerate(pairs):
                if pi_ + 1 < npr:
                    emit_S(pi_ + 1)
                b0 = 2 * (pi_ % 2)
                n2 = len(pr)
                src = pa3[:, b0:b0 + n2, 0:nq]
                skeys = [("pa", b0 + idx) for idx in range(n2)]
                Et, Ek = Er.next()
                masked = masks is not None and pr[0] in masks
                if masked:
                    e0, e0k = e0r.next()
                    P.op("act", lambda e: e.activation(out=e0[:, 0:n2, 0:nq], in_=src, func=AF.Exp, scale=scale), reads=skeys, writes=[e0k])
                    for idx, kt in enumerate(pr):
                        W, Wk = masks[kt]
                        P.op("pool", lambda e: e.tensor_tensor(out=Et[:, idx, 0:nq], in0=e0[:, idx, 0:nq], in1=W, op=ALU.mult), reads=[e0k, Wk], writes=[Ek])
                else:
                    P.op("act", lambda e: e.activation(out=Et[:, 0:n2, 0:nq], in_=src, func=AF.Exp, scale=scale), reads=skeys, writes=[Ek])
                for idx, kt in enumerate(pr):
                    P.op("pe", lambda e: e.matmul(OB, lhsT=vt[:, kt, :], rhs=Et[:, idx, 0:nq], start=(step == 0), stop=(step == nk - 1)),
                         reads=[(vk, kt), Ek], writes=[obk])
                    P.op("pe", lambda e: e.matmul(DB, lhsT=onesb[:], rhs=Et[:, idx, 0:nq], start=(step == 0), stop=(step == nk - 1)),
                         reads=["onesb", Ek], writes=[dbk])
                    step += 1
            rd, rdk = f32r.next()
            P.op("dve", lambda e: e.reciprocal(out=rd[:, 0:nq], in_=DB), reads=[dbk], writes=[rdk])
            o, ok = f32r.next()
            P.op("dve", lambda e: e.tensor_tensor(out=o[:, 0:nq], in0=OB, in1=rd[:, 0:nq], op=ALU.mult), reads=[obk, rdk], writes=[ok])
            return o, ok

        def emit_o(chunk, src_ap, src_keys, q0, nq, scale_col=None):
            ot, otk = ofm.next()
            if scale_col is None:
                P.op("act", lambda e: e.copy(out=ot[:, 0:nq], in_=src_ap), reads=src_keys, writes=[otk])
            P.dma("sp", o_scr[chunk, :, q0:q0 + nq], ot[:, 0:nq], reads=[otk], writes=[("o", chunk, q0)])

        def tok_keys(name):
            return lambda q0, nq: (name, q0 // 128)

        whole = lambda key: (lambda *a: key)

        def is_lat(tok0):
            return tok0 >= CTX

        def mixer_na():
            P.set_phase(f"L{l}.na.setup")
            b0 = A.off
            Gexp = A.alloc("Gexp", [128, 4, 15, 64], F32)
            cmask = A.alloc("cmask", [128, 64], F32)
            Wt = A.alloc("Wt", [128, 24, 128], F32)
            P.dma("sp", cmask[:], c_cmask, writes=["cmask"])
            for a in range(2):
                P.dma("sp", Gexp[a * 64:(a + 1) * 64], na_bias[l].rearrange("h r k q -> k h r q"), writes=["Gexp"])
            Gf = Gexp[:].rearrange("p h r q -> p (h r) q")
            P.op("act", lambda e: e.activation(out=Gexp[:].rearrange("p h r q -> p (h r q)"), in_=Gexp[:].rearrange("p h r q -> p (h r q)"), func=AF.Exp),
                 reads=["Gexp"], writes=["Gexp"])
            P.op("dve", lambda e: e.tensor_tensor(out=Gf, in0=Gf, in1=cmask[:].unsqueeze(1).to_broadcast([128, 60, 64]), op=ALU.mult),
                 reads=["Gexp", "cmask"], writes=["Gexp"])
            rows = SEQ // GRID_W
            rs_of = lambda r: min(max(r - 4, 0), rows - 8)
            for h in range(4):
                P.set_phase(f"L{l}.na.proj")
                wt, wk, offs = load_w([(OFF["na_q"] + h * 128, 128), (OFF["na_k"] + h * 128, 128), (OFF["na_v"] + h * 128, 128)])
                q_t, qk = qT.next()
                k_t, kk = kT.next()
                v_t, vk = vv.next()
                for (t0, n) in all_blocks:
                    for (dst, dk, o) in ((q_t, qk, offs[0]), (k_t, kk, offs[1])):
                        if last and dst is q_t and not is_lat(t0):
                            continue
                        j = proj_fm(wt, wk, o, 128, t0, n)
                        P.op("act", lambda e: e.copy(out=dst[:, t0:t0 + n], in_=PB(j, 128, n)), reads=[("pb", j)], writes=[dk])
                P.set_phase(f"L{l}.na.v")
                proj_v(wt, wk, offs[2], v_t, vk)
                P.set_phase(f"L{l}.na.att")
                wcache = {}
                nW = [0]

                def get_W(r0, k0):
                    blocks = []
                    for a in range(2):
                        for b in range(2):
                            kr, r = k0 + a, r0 + b
                            ok = rs_of(r) <= kr < rs_of(r) + 8
                            blocks.append((kr - r + 7) if ok else None)
                    sig = tuple(blocks)
                    if sig in wcache:
                        return wcache[sig]
                    idx = nW[0]
                    nW[0] += 1
                    assert idx < 24
                    key = ("Wt", idx)
                    if any(bk is None for bk in blocks):
                        P.op("pool", lambda e: e.memset(Wt[:, idx, :], 0.0), writes=[key])
                    for a in range(2):
                        for b in range(2):
                            dr = blocks[a * 2 + b]
                            if dr is None:
                                continue
                            P.op("pool", lambda e: e.tensor_copy(out=Wt[a * 64:(a + 1) * 64, idx, b * 64:(b + 1) * 64],
                                                                 in_=Gexp[a * 64:(a + 1) * 64, h, dr, :]),
                                 reads=["Gexp"], writes=[key])
                    wcache[sig] = (Wt[:, idx, :], key)
                    return wcache[sig]

                qp = [(q_t, whole(qk), 0, 128)]
                kp = [(k_t, whole(kk), 0, 128)]
                for qt_i in range(16):
                    r0 = 2 * qt_i
                    lo = min(rs_of(r0), rs_of(r0 + 1))
                    hi = max(rs_of(r0), rs_of(r0 + 1)) + 8
                    k0s = list(range(lo - (lo % 2), hi, 2))
                    masks = {2 + k0 // 2: get_W(r0, k0) for k0 in k0s}
                    key_tiles = [0, 1] + [2 + k0 // 2 for k0 in k0s]
                    o, ok = attend(qp, kp, v_t, vk, CTX + qt_i * 128, 128, key_tiles, NA_SCALE, masks)
                    emit_o(h, o[:, 0:128], [ok], CTX + qt_i * 128, 128)
                if not last:
                    o, ok = attend(qp, kp, v_t, vk, 0, CTX, [0, 1], NA_SCALE)
                    emit_o(h, o[:, 0:CTX], [ok], 0, CTX)
            P.barrier()
            A.off = b0

        def load_rope(which):
            i0 = 0 if which == 128 else 2
            ts = []
            for i in range(2):
                t = A.alloc(f"rope{which}_{i}", [128, SEQ], F32)
                P.dma("sp", t[:], c_rope[i0 + i], writes=["rope"])
                ts.append(t)
            rope_t[which] = tuple(ts)
            pm = A.alloc(f"perm{which}", [128, 128], F32)
            P.dma("sp", pm[:], c_perm32 if which == 128 else c_perm16, writes=["perm32" if which == 128 else "perm16"])
            return pm

        def load_gcol(col, src_row_ap, n):
            with nc.allow_non_contiguous_dma(reason="tiny per-partition gain column"):
                P.dma("sp", gcol[0:n, col:col + 1], src_row_ap.rearrange("o n -> n o"), writes=[("gcol", col)])

        def mixer_gqa(p16, p32):
            P.set_phase(f"L{l}.gqa.proj")
            load_gcol(0, gqa_g_q[l:l + 1, :], 128)
            load_gcol(1, gqa_g_k[l:l + 1, :], 128)

            def normed_roped(wt, wk, o, gcolidx, dst, dk, t0, n):
                j = proj_fm(wt, wk, o, 128, t0, n)
                raw, rk = f32r.next()
                P.op("act", lambda e: e.copy(out=raw[:, 0:n], in_=PB(j, 128, n)), reads=[("pb", j)], writes=[rk])
                rs, rsk = mean_rstd(lambda c: raw[:, 0:n], [rk], 128, n)
                z, zk = f32r.next()
                P.op("dve", lambda e: e.scalar_tensor_tensor(out=z[:, 0:n], in0=raw[:, 0:n], scalar=gcol[:, gcolidx:gcolidx + 1], in1=rs[:, 0:n],
                                                              op0=ALU.mult, op1=ALU.mult), reads=[rk, rsk, ("gcol", gcolidx)], writes=[zk])
                if is_lat(t0):
                    rope_apply(z, zk, 128, t0, n, 128, p32, "perm32", dst[:, t0:t0 + n], dk)
                else:
                    P.op("act", lambda e: e.copy(out=dst[:, t0:t0 + n], in_=z[:, 0:n]), reads=[zk], writes=[dk])

            for g in range(2):
                wt, wk, offs = load_w([(OFF["gqa_k"] + g * 128, 128), (OFF["gqa_v"] + g * 128, 128)])
                k_t, kk = kT.next()
                v_t, vk = vv.next()
                for (t0, n) in all_blocks:
                    normed_roped(wt, wk, offs[0], 1, k_t, kk, t0, n)
                P.set_phase(f"L{l}.gqa.v")
                proj_v(wt, wk, offs[1], v_t, vk)
                for hh in range(2):
                    h = 2 * g + hh
                    wq, wqk, qoffs = load_w([(OFF["gqa_q"] + h * 128, 128)])
                    q_t, qk = qT.next()
                    P.set_phase(f"L{l}.gqa.proj")
                    for (t0, n) in q_blocks:
                        normed_roped(wq, wqk, qoffs[0], 0, q_t, qk, t0, n)
                    P.set_phase(f"L{l}.gqa.att")
                    qp = [(q_t, whole(qk), 0, 128)]
                    kp = [(k_t, whole(kk), 0, 128)]
                    for (q0, nq) in q_blocks:
                        kts = list(range(NT)) if is_lat(q0) else [0, 1]
                        o, ok = attend(qp, kp, v_t, vk, q0, nq, kts, GQA_SCALE)
                        emit_o(8 + h, o[:, 0:nq], [ok], q0, nq)

        def mixer_diff(p16, p32):
            P.set_phase(f"L{l}.diff.proj")
            load_gcol(2, diff_g_sub[l:l + 1, :], 128)
            lamt = A.alloc("lamt", [128, 4, 64], F32)
            lamc = A.alloc("lamc", [128, 4], F32)
            P.dma("sp", lamt[:].rearrange("p a n -> p (a n)"), dlam[l:l + 1].rearrange("o a n -> o (a n)").partition_broadcast(128), writes=["lamt"])
            for i in range(2):
                P.op("dve", lambda e: e.scalar_tensor_tensor(out=lamt[:, 2 * i, :], in0=lamt[:, 2 * i, :], scalar=1.0, in1=lamt[:, 2 * i + 1, :],
                                                              op0=ALU.mult, op1=ALU.mult, accum_out=lamc[:, i:i + 1]),
                     reads=["lamt"], writes=["lamt", "lamc"])
            P.op("act", lambda e: e.activation(out=lamc[:, 0:2], in_=lamc[:, 0:2], func=AF.Exp), reads=["lamc"], writes=["lamc"])
            P.op("dve", lambda e: e.tensor_tensor(out=lamc[:, 2:3], in0=lamc[:, 1:2], in1=lamc[:, 0:1], op=ALU.subtract), reads=["lamc"], writes=["lamc"])
            P.op("dve", lambda e: e.tensor_scalar(out=lamc[:, 2:3], in0=lamc[:, 2:3], scalar1=-lam_init, scalar2=None, op0=ALU.add), reads=["lamc"], writes=["lamc"])
            P.op("dve", lambda e: e.tensor_scalar(out=gcol[:, 3:4], in0=gcol[:, 2:3], scalar1=(1.0 - lam_init), scalar2=None, op0=ALU.mult),
                 reads=[("gcol", 2)], writes=[("gcol", 3)])
            for h in range(4):
                P.set_phase(f"L{l}.diff.proj")
                wt, wk, offs = load_w([(OFF["diff_q"] + h * 128, 128), (OFF["diff_k"] + h * 128, 128), (OFF["diff_v"] + h * 128, 128)])
                q_t, qk = qT.next()
                k_t, kk = kT.next()
                v_t, vk = vv.next()
                for (t0, n) in all_blocks:
                    for (dst, dk, o) in ((q_t, qk, offs[0]), (k_t, kk, offs[1])):
                        if last and dst is q_t and not is_lat(t0):
                            continue
                        j = proj_fm(wt, wk, o, 128, t0, n)
                        if is_lat(t0):
                            z, zk = f32r.next()
                            P.op("act", lambda e: e.copy(out=z[:, 0:n], in_=PB(j, 128, n)), reads=[("pb", j)], writes=[zk])
                            rope_apply(z, zk, 128, t0, n, 64, p16, "perm16", dst[:, t0:t0 + n], dk)
                        else:
                            P.op("act", lambda e: e.copy(out=dst[:, t0:t0 + n], in_=PB(j, 128, n)), reads=[("pb", j)], writes=[dk])
                P.set_phase(f"L{l}.diff.v")
                proj_v(wt, wk, offs[2], v_t, vk)
                P.set_phase(f"L{l}.diff.att")
                for (q0, nq) in q_blocks:
                    kts = list(range(NT)) if is_lat(q0) else [0, 1]
                    o1, o1k = attend([(q_t, whole(qk), 0, 64)], [(k_t, whole(kk), 0, 64)], v_t, vk, q0, nq, kts, DIFF_SCALE)
                    o2, o2k = attend([(q_t, whole(qk), 64, 128)], [(k_t, whole(kk), 64, 128)], v_t, vk, q0, nq, kts, DIFF_SCALE)
                    od, odk = f32r.next()
                    P.op("dve", lambda e: e.scalar_tensor_tensor(out=od[:, 0:nq], in0=o2[:, 0:nq], scalar=lamc[:, 2:3], in1=o1[:, 0:nq],
                                                                  op0=ALU.mult, op1=ALU.add), reads=[o1k, o2k, "lamc"], writes=[odk])
                    rs, rsk = mean_rstd(lambda c: od[:, 0:nq], [odk], 128, nq)
                    ot, otk = ofm.next()
                    P.op("dve", lambda e: e.scalar_tensor_tensor(out=ot[:, 0:nq], in0=od[:, 0:nq], scalar=gcol[:, 3:4], in1=rs[:, 0:nq],
                                                                  op0=ALU.mult, op1=ALU.mult), reads=[odk, rsk, ("gcol", 3)], writes=[otk])
                    P.dma("sp", o_scr[12 + h, :, q0:q0 + nq], ot[:, 0:nq], reads=[otk], writes=[("o", 12 + h, q0)])

        def mixer_mla(p16, p32):
            P.set_phase(f"L{l}.mla.proj")
            b0 = A.off
            cqn = A.alloc("cqn", [128, 3, NTOK], BF16)
            ckvn = A.alloc("ckvn", [128, NTOK], BF16)
            kpe = A.alloc("kpe", [64, NTOK], BF16)
            qpe = Ring(A, "qpe", 1, [64, NTOK], BF16)
            wqup = A.alloc("wqup", [128, 3, 768], BF16)
            wkvup = A.alloc("wkvup", [128, 1024], BF16)
            raw3 = A.alloc("raw3", [128, 3, 512], F32)
            P.dma("pool", wqup[:], mla_w_qup[l].rearrange("(c p) n -> p c n", p=128), writes=["wqup"])
            P.dma("pool", wkvup[:], mla_w_kvup[l], writes=["wkvup"])
            for c in range(3):
                load_gcol(4 + c, mla_g_q[l:l + 1, c * 128:(c + 1) * 128], 128)
            load_gcol(7, mla_g_kv[l:l + 1, :], 128)
            wt, wk, offs = load_w([(OFF["mla_cq"], 384)])
            for (t0, n) in q_blocks:
                for c in range(3):
                    j = proj_fm(wt, wk, offs[0] + c * 128, 128, t0, n)
                    P.op("act", lambda e: e.copy(out=raw3[:, c, 0:n], in_=PB(j, 128, n)), reads=[("pb", j)], writes=[("raw3", c)])
                rs, rsk = mean_rstd(lambda c: raw3[:, c, 0:n], [("raw3", c) for c in range(3)], 128, n, nchunks=3, denom=384.0)
                for c in range(3):
                    P.op("dve", lambda e: e.scalar_tensor_tensor(out=cqn[:, c, t0:t0 + n], in0=raw3[:, c, 0:n], scalar=gcol[:, 4 + c:5 + c], in1=rs[:, 0:n],
                                                                  op0=ALU.mult, op1=ALU.mult), reads=[("raw3", c), rsk, ("gcol", 4 + c)], writes=["cqn"])
            wt, wk, offs = load_w([(OFF["mla_ckv"], 128), (OFF["mla_kpe"], 64)])
            for (t0, n) in all_blocks:
                j = proj_fm(wt, wk, offs[0], 128, t0, n)
                raw, rk = f32r.next()
                P.op("act", lambda e: e.copy(out=raw[:, 0:n], in_=PB(j, 128, n)), reads=[("pb", j)], writes=[rk])
                rs, rsk = mean_rstd(lambda c: raw[:, 0:n], [rk], 128, n)
                P.op("dve", lambda e: e.scalar_tensor_tensor(out=ckvn[:, t0:t0 + n], in0=raw[:, 0:n], scalar=gcol[:, 7:8], in1=rs[:, 0:n],
                                                              op0=ALU.mult, op1=ALU.mult), reads=[rk, rsk, ("gcol", 7)], writes=["ckvn"])
                j = proj_fm(wt, wk, offs[1], 64, t0, n)
                if is_lat(t0):
                    z, zk = f32r.next()
                    P.op("act", lambda e: e.copy(out=z[0:64, 0:n], in_=PB(j, 64, n)), reads=[("pb", j)], writes=[zk])
                    rope_apply(z, zk, 64, t0, n, 64, p16, "perm16", kpe[:, t0:t0 + n], "kpe")
                else:
                    P.op("act", lambda e: e.copy(out=kpe[:, t0:t0 + n], in_=PB(j, 64, n)), reads=[("pb", j)], writes=["kpe"])
            for h in range(4):
                P.set_phase(f"L{l}.mla.proj")
                q_t, qk = qT.next()
                k_t, kk = kT.next()
                v_t, vk = vv.next()
                qp_t, qpk = qpe.next()
                for (t0, n) in all_blocks:
                    j = next_pb()
                    P.op("pe", lambda e: e.matmul(PB(j, 128, n), lhsT=wkvup[:, h * 256:h * 256 + 128], rhs=ckvn[:, t0:t0 + n], start=True, stop=True),
                         reads=["wkvup", "ckvn"], writes=[("pb", j)])
                    P.op("act", lambda e: e.copy(out=k_t[:, t0:t0 + n], in_=PB(j, 128, n)), reads=[("pb", j)], writes=[kk])
                for t in range(NT):
                    j = next_pb()
                    P.op("pe", lambda e: e.matmul(PB(j, 128, 128), lhsT=ckvn[:, t * 128:(t + 1) * 128], rhs=wkvup[:, h * 256 + 128:h * 256 + 256], start=True, stop=True),
                         reads=["wkvup", "ckvn"], writes=[("pb", j)])
                    P.op("act", lambda e: e.copy(out=v_t[:, t, :], in_=PB(j, 128, 128)), reads=[("pb", j)], writes=[(vk, t)])
                for (t0, n) in q_blocks:
                    j = next_pb()
                    for c in range(3):
                        P.op("pe", lambda e: e.matmul(PB(j, 128, n), lhsT=wqup[:, c, h * 192:h * 192 + 128], rhs=cqn[:, c, t0:t0 + n], start=(c == 0), stop=(c == 2)),
                             reads=["wqup", "cqn"], writes=[("pb", j)])
                    P.op("act", lambda e: e.copy(out=q_t[:, t0:t0 + n], in_=PB(j, 128, n)), reads=[("pb", j)], writes=[qk])
                    j = next_pb()
                    for c in range(3):
                        P.op("pe", lambda e: e.matmul(PB(j, 64, n), lhsT=wqup[:, c, h * 192 + 128:h * 192 + 192], rhs=cqn[:, c, t0:t0 + n], start=(c == 0), stop=(c == 2)),
                             reads=["wqup", "cqn"], writes=[("pb", j)])
                    if is_lat(t0):
                        z, zk = f32r.next()
                        P.op("act", lambda e: e.copy(out=z[0:64, 0:n], in_=PB(j, 64, n)), reads=[("pb", j)], writes=[zk])
                        rope_apply(z, zk, 64, t0, n, 64, p16, "perm16", qp_t[:, t0:t0 + n], qpk)
                    else:
                        P.op("act", lambda e: e.copy(out=qp_t[:, t0:t0 + n], in_=PB(j, 64, n)), reads=[("pb", j)], writes=[qpk])
                P.set_phase(f"L{l}.mla.att")
                qp = [(q_t, whole(qk), 0, 128), (qp_t, whole(qpk), 0, 64)]
                kp = [(k_t, whole(kk), 0, 128), (kpe, whole("kpe"), 0, 64)]
                for (q0, nq) in q_blocks:
                    kts = list(range(NT)) if is_lat(q0) else [0, 1]
                    o, ok = attend(qp, kp, v_t, vk, q0, nq, kts, MLA_SCALE)
                    emit_o(4 + h, o[:, 0:nq], [ok], q0, nq)
            P.barrier()
            A.off = b0

        mixer_na()
        b1 = A.off
        p16 = load_rope(64)
        mixer_mla(p16, None)
        mixer_diff(p16, None)
        P.barrier()
        A.off = b1
        p32 = load_rope(128)
        mixer_gqa(None, p32)
        P.barrier()
        A.off = lay_base
        if dbg == f"o{l}":
            d = dbg_tensor("o", [16, 128, NTOK], BF16)
            ot_ = A.alloc("dbgo", [128, NTOK], BF16)
            for c in range(16):
                P.dma("sp", ot_[:], o_scr[c], writes=["dbgo"])
                P.dma("sp", d[c], ot_[:], reads=["dbgo"], writes=["dbg"])
            raise _Stop()

        P.set_phase(f"L{l}.D")
        tiles = list(range(2, NT)) if last else list(range(NT))
        base = A.off
        u2st = Ring(A, "u2st", 2, [128, 16, 128], BF16)
        wo = A.alloc("wo", [128, 16, D], BF16)
        wosrc = w_out[l].rearrange("(k p) n -> p k n", p=128)
        for c4 in range(4):
            P.dma("pool", wo[:, 4 * c4:4 * c4 + 4, :], wosrc[:, 4 * c4:4 * c4 + 4, :], writes=["wo"])
        nb = make_norm_bufs(2)
        gf = A.alloc("gf", [128, D], F32)
        bcast_row(gf[:], g_ffn[l:l + 1, :], "gf")
        mtd = {}
        for slot in (2, 3, 4):
            mtd[slot] = (A.alloc(f"mtd{slot}", [128, D], F32), ("mtd", slot))

        def load_mtd(who):
            for slot in (2, 3, 4):
                bcast_row(mtd[slot][0][:], modv[l, who:who + 1, slot * D:(slot + 1) * D], mtd[slot][1])
            sc, sck = mtd[4]
            P.op("dve", lambda e: e.scalar_tensor_tensor(out=sc[:], in0=sc[:], scalar=1.0, in1=gf[:], op0=ALU.add, op1=ALU.mult),
                 reads=[sck, "gf"], writes=[sck])

        oTr = Ring(A, "oTr", 2, [128, 16, 128], BF16)
        hm = Ring(A, "hm", 2, [128, D], F32)
        if last:
            wr32 = A.alloc("wr32", [128, 16, NEXP], F32)
            br32 = A.alloc("br32", [1, NEXP], F32)
            f32T = Ring(A, "f32T", 2, [128, 16, 128], F32)
            P.dma("sp", wr32[:], moe_wr[0].rearrange("(k p) n -> p k n", p=128), writes=["wr32"])
            P.dma("sp", br32[:], moe_br[0:1, :], writes=["br32"])
        rt = Ring(A, "rt", 2, [128, 4 * NEXP + 8], F32) if last else None
        mt = {(w, s_): mtd[s_] for w in (0, 1) for s_ in (2, 3, 4)}
        cur_who = [None]
        pending = [None]
        for t in tiles:
            who = 1 if t < 2 else 0
            if cur_who[0] != who:
                load_mtd(who)
                cur_who[0] = who
            u2T, u2k = u2st.next()
            ot, otk = oTr.next()
            P.dma("sp", ot[:], o_scr[:, :, t * 128:(t + 1) * 128].rearrange("c p n -> p c n"),
                  reads=[("o", c, q0) for c in range(16) for (q0, nq) in q_blocks if q0 <= t * 128 < q0 + nq], writes=[otk])
            for jb in range(4):
                for c in range(16):
                    P.op("pe", lambda e: e.matmul(PB(jb), lhsT=ot[:, c, :], rhs=wo[:, c, jb * 512:(jb + 1) * 512], start=(c == 0), stop=(c == 15)),
                         reads=[otk, "wo"], writes=[("pb", jb)])
            xt, xk = nb["xt"].next()
            src, sk = h_src(l, t)
            P.dma("sp", xt[:], src, reads=[sk] if sk else [], writes=[xk])
            hmt, hmk = hm.next()
            gt, gtk = mt[(who, 2)]
            P.op("dve", lambda e: e.tensor_tensor(out=hmt[:], in0=pb[:], in1=gt[:], op=ALU.mult), reads=[("pb", j) for j in range(4)] + [gtk], writes=[hmk])
            P.op("pool", lambda e: e.tensor_tensor(out=hmt[:], in0=hmt[:], in1=xt[:], op=ALU.add), reads=[hmk, xk], writes=[hmk])
            P.dma("sp", h_scr[t * 128:(t + 1) * 128, :], hmt[:], reads=[hmk], writes=[("h", t)])
            ft, ftk = f32T.next() if last else (None, None)
            partB = norm_core(nb, hmt, hmk, mt[(who, 4)][0], mt[(who, 4)][1], mt[(who, 3)][0], mt[(who, 3)][1], u2T, u2k, 0,
                              want_f32T=((ft, ftk) if last else None), defer=True)

            def tail(t=t, u2T=u2T, u2k=u2k, ft=ft, ftk=ftk, partB=partB):
                partB()
                P.dma("sp", u2_scr[:, :, t * 128:(t + 1) * 128].rearrange("k p n -> p k n"), u2T[:], reads=[u2k], writes=[("u2", t)])
                if not last:
                    return
                for k in range(16):
                    P.op("pe", lambda e: e.matmul(PA(0, 128, NEXP), lhsT=ft[:, k, :], rhs=wr32[:, k, :], start=(k == 0), stop=False),
                         reads=[ftk, "wr32"], writes=[("pa", 0)])
                P.op("pe", lambda e: e.matmul(PA(0, 128, NEXP), lhsT=ones1[0:1, :], rhs=br32[:], start=False, stop=True),
                     reads=["ones1", "br32"], writes=[("pa", 0)])
                r, rk = rt.next()
                lg, m1, mk1, l2, m2, mk2, ee = r[:, 0:8], r[:, 32:33], r[:, 8:16], r[:, 16:24], r[:, 33:34], r[:, 24:32], r[:, 34:35]
                dd, rr, g2 = r[:, 35:36], r[:, 36:37], r[:, 37:38]
                ti = t - 2
                V = lambda f: P.op("dve", f, reads=[rk], writes=[rk])
                P.op("dve", lambda e: e.tensor_copy(out=lg, in_=PA(0, 128, NEXP)), reads=[("pa", 0)], writes=[rk])
                V(lambda e: e.reduce_max(out=m1, in_=lg, axis=mybir.AxisListType.X))
                V(lambda e: e.tensor_scalar(out=mk1, in0=lg, scalar1=m1, scalar2=None, op0=ALU.is_equal))
                V(lambda e: e.scalar_tensor_tensor(out=l2, in0=mk1, scalar=-1e30, in1=lg, op0=ALU.mult, op1=ALU.add))
                V(lambda e: e.reduce_max(out=m2, in_=l2, axis=mybir.AxisListType.X))
                V(lambda e: e.tensor_scalar(out=mk2, in0=l2, scalar1=m2, scalar2=None, op0=ALU.is_equal))
                V(lambda e: e.tensor_tensor(out=dd, in0=m2, in1=m1, op=ALU.subtract))
                P.op("act", lambda e: e.activation(out=ee, in_=dd, func=AF.Exp), reads=[rk], writes=[rk])
                V(lambda e: e.tensor_scalar(out=rr, in0=ee, scalar1=1.0, scalar2=None, op0=ALU.add))
                V(lambda e: e.reciprocal(out=rr, in_=rr))
                V(lambda e: e.tensor_tensor(out=g2, in0=ee, in1=rr, op=ALU.mult))
                V(lambda e: e.tensor_scalar(out=mk1, in0=mk1, scalar1=rr, scalar2=None, op0=ALU.mult))
                P.op("dve", lambda e: e.scalar_tensor_tensor(out=comb[:, ti, :], in0=mk2, scalar=g2, in1=mk1, op0=ALU.mult, op1=ALU.add),
                     reads=[rk], writes=[("comb", ti)])

            if pending[0] is not None:
                pending[0]()
            pending[0] = tail
        if pending[0] is not None:
            pending[0]()
        P.barrier()
        if dbg == f"h{l}":
            d = dbg_tensor("h", [NTOK, D], F32)
            for t in tiles:
                xt, xk = nb["xt"].next()
                P.dma("sp", xt[:], h_scr[t * 128:(t + 1) * 128, :], writes=[xk])
                P.dma("sp", d[t * 128:(t + 1) * 128, :], xt[:], reads=[xk], writes=["dbg"])
            raise _Stop()
        if dbg == f"u2T{l}":
            d = dbg_tensor("u2T", [16, 128, NTOK], BF16)
            P.dma("sp", d, u2_scr, writes=["dbg"])
            if last:
                d2 = dbg_tensor("comb", [128, 16, NEXP], F32)
                P.dma("sp", d2, comb[:], writes=["dbg2"])
            raise _Stop()
        P.set_phase(f"L{l}.ffn")
        comb_keep = comb
        A.off = base
        mt = {}
        for who in ((0,) if last else (0, 1)):
            tt_ = A.alloc(f"gtf{who}", [128, D], F32)
            bcast_row(tt_[:], modv[l, who:who + 1, 5 * D:6 * D], ("gtf", who))
            mt[(who, 5)] = (tt_, ("gtf", who))
        gfin = None
        if last:
            gfin = A.alloc("gfin", [128, D], F32)
            bcast_row(gfin[:], g_final[0:1, :], "gfin")
        G = 2
        wgu = Ring(A, "wgu", 3 if last else 2, [128, 2, 16, G * 128], BF16)
        wdr = Ring(A, "wdr", 2, [128, G, D], BF16)
        if last:
            passes = [(CTX, 1024), (CTX + 1024, 1024)]
            experts = [(moe_wg[0, e], moe_wu[0, e], moe_wd[0, e], e) for e in range(NEXP)]
            F = DFFE
        else:
            passes = [(0, 768), (768, 768), (1536, 768)]
            experts = [(ffn_wg[0], ffn_wu[0], ffn_wd[0], None)]
            F = DFF
        TBMAX = max(n for _, n in passes)
        y = A.alloc("y", [128, TBMAX // 128, D], F32)
        Hr = Ring(A, "H", 2, [128, G, TBMAX], BF16)
        sr = Ring(A, "s", 3, [128, 512], F32)
        fjunk = A.alloc("fjunk", [128, D], BF16) if last else None
        fss = Ring(A, "fss", 2, [128, 2], F32) if last else None
        hx = Ring(A, "hx", 1 if last else 2, [128, D], F32)
        u2pr = Ring(A, "u2p", 1, [128, 16, TBMAX], BF16)
        pdi = [0]
        for (p0, pn) in passes:
            ntile = pn // 128
            u2T, u2pk = u2pr.next()
            P.dma("sp", u2T[:, :, 0:pn], u2_scr[:, :, p0:p0 + pn].rearrange("k p n -> p k n"), writes=[u2pk])
            tbs = [(i, min(512, pn - i)) for i in range(0, pn, 512)]
            first_write = [True] * (ntile * 4)
            gi = [0]
            groups = []
            for (wg_, wu_, wd_, eidx) in experts:
                wgs = wg_.rearrange("(k p) n -> p k n", p=128)
                wus = wu_.rearrange("(k p) n -> p k n", p=128)
                wds = wd_.rearrange("(f p) n -> p f n", p=128)
                for fg in range(F // (G * 128)):
                    groups.append((wgs, wus, wds, eidx, fg))
            units = [(tb0, tbn, fc) for (tb0, tbn) in tbs for fc in range(G)]
            dtiles = [(tt, jb) for tt in range(ntile) for jb in range(4)]
            nd = len(dtiles)
            nu = len(units)

            def gate_up(wt, wk, Ht, Hk, unit, prev=None, lo=0, hi=0):
                tb0, tbn, fc = unit
                par = gi[0] % 2
                gi[0] += 1
                nmm = 0
                dpos = lo
                ndn = hi - lo
                for w in range(2):
                    bank = 2 * par + w
                    for k in range(16):
                        P.op("pe", lambda e: e.matmul(PA(bank, 128, tbn), lhsT=wt[:, w, k, fc * 128:(fc + 1) * 128],
                                                      rhs=u2T[:, k, tb0:tb0 + tbn], start=(k == 0), stop=(k == 15)),
                             reads=[wk, u2pk], writes=[("pa", bank)])
                        nmm += 1
                        if prev is not None and ndn > 0:
                            want = lo + (ndn * nmm) // 32
                            if want > dpos:
                                down(prev, dpos, want)
                                dpos = want
                st, stk = sr.next()
                P.op("act", lambda e: e.activation(out=st[:, 0:tbn], in_=PA(2 * par, 128, tbn), func=AF.Silu), reads=[("pa", 2 * par)], writes=[stk])
                P.op("dve", lambda e: e.tensor_tensor(out=Ht[:, fc, tb0:tb0 + tbn], in0=st[:, 0:tbn], in1=PA(2 * par + 1, 128, tbn), op=ALU.mult),
                     reads=[stk, ("pa", 2 * par + 1)], writes=[(Hk, fc, tb0)])

            def down(prev, lo, hi):
                Ht, Hk, wdt, wdk, eidx = prev
                for (tt, jb) in dtiles[lo:hi]:
                    tb0 = (tt * 128 // 512) * 512
                    j = pdi[0] % 4
                    pdi[0] += 1
                    for fc in range(G):
                        P.op("pe", lambda e: e.matmul(PB(j), lhsT=Ht[:, fc, tt * 128:(tt + 1) * 128], rhs=wdt[:, fc, jb * 512:(jb + 1) * 512],
                                                      start=(fc == 0), stop=(fc == G - 1)),
                             reads=[(Hk, fc, tb0), wdk], writes=[("pb", j)])
                    ysl = y[:, tt, jb * 512:(jb + 1) * 512]
                    yk = ("y", tt, jb)
                    fw = first_write[tt * 4 + jb]
                    first_write[tt * 4 + jb] = False
                    if eidx is None:
                        if fw:
                            P.op("dve", lambda e: e.tensor_copy(out=ysl, in_=PB(j)), reads=[("pb", j)], writes=[yk])
                        else:
                            P.op("dve", lambda e: e.tensor_tensor(out=ysl, in0=PB(j), in1=ysl, op=ALU.add), reads=[("pb", j), yk], writes=[yk])
                    else:
                        ccol = comb_keep[:, (p0 - CTX) // 128 + tt, eidx:eidx + 1]
                        if fw:
                            P.op("dve", lambda e: e.tensor_scalar(out=ysl, in0=PB(j), scalar1=ccol, scalar2=None, op0=ALU.mult),
                                 reads=[("pb", j), "comb_keep"], writes=[yk])
                        else:
                            P.op("dve", lambda e: e.scalar_tensor_tensor(out=ysl, in0=PB(j), scalar=ccol, in1=ysl, op0=ALU.mult, op1=ALU.add),
                                 reads=[("pb", j), yk, "comb_keep"], writes=[yk])

            prev = None
            for (wgs, wus, wds, eidx, fg) in groups:
                wt, wk = wgu.next()
                P.dma("pool", wt[:, 0], wgs[:, :, fg * G * 128:(fg + 1) * G * 128], writes=[wk])
                P.dma("pool", wt[:, 1], wus[:, :, fg * G * 128:(fg + 1) * G * 128], writes=[wk])
                wdt, wdk = wdr.next()
                P.dma("pool", wdt[:], wds[:, fg * G:(fg + 1) * G, :], writes=[wdk])
                Ht, Hk = Hr.next()
                for ui, unit in enumerate(units):
                    gate_up(wt, wk, Ht, Hk, unit, prev, nd * ui // nu, nd * (ui + 1) // nu)
                prev = (Ht, Hk, wdt, wdk, eidx)
            down(prev, 0, nd)
            for tt in range(ntile):
                t = (p0 // 128) + tt
                who = 1 if t < 2 else 0
                ht, hk = hx.next()
                P.dma("sp", ht[:], h_scr[t * 128:(t + 1) * 128, :], reads=[("h", t)], writes=[hk])
                gt, gtk = mt[(who, 5)]
                yk4 = [("y", tt, jb) for jb in range(4)]
                P.op("dve", lambda e: e.tensor_tensor(out=y[:, tt, :], in0=y[:, tt, :], in1=gt[:], op=ALU.mult), reads=yk4 + [gtk], writes=yk4)
                P.op("dve", lambda e: e.tensor_tensor(out=ht[:], in0=ht[:], in1=y[:, tt, :], op=ALU.add), reads=yk4 + [hk], writes=[hk])
                if not last:
                    P.dma("sp", h_scr[t * 128:(t + 1) * 128, :], ht[:], reads=[hk], writes=[("h", t)])
                else:
                    sst, ssk = fss.next()
                    P.op("dve", lambda e: e.scalar_tensor_tensor(out=fjunk[:], in0=ht[:], scalar=1.0, in1=ht[:], op0=ALU.mult, op1=ALU.mult,
                                                                  accum_out=sst[:, 0:1]), reads=[hk], writes=["fjunk", ssk])
                    P.op("act", lambda e: e.activation(out=sst[:, 1:2], in_=sst[:, 0:1], func=AF.Sqrt, bias=epsb[:, 0:1], scale=1.0 / D),
                         reads=[ssk, "epsb"], writes=[ssk])
                    P.op("dve", lambda e: e.reciprocal(out=sst[:, 1:2], in_=sst[:, 1:2]), reads=[ssk], writes=[ssk])
                    P.op("dve", lambda e: e.scalar_tensor_tensor(out=y[:, tt, :], in0=ht[:], scalar=sst[:, 1:2], in1=gfin[:], op0=ALU.mult, op1=ALU.mult),
                         reads=[hk, ssk, "gfin"] + yk4, writes=yk4)
                    P.dma("sp", out[(t - 2) * 128:(t - 1) * 128, :], y[:, tt, :], reads=yk4, writes=[("out", t)])
        P.barrier()
        A.off = lay_base

    phase_mod()
    try:
        for l in range(2):
            layer(l)
            if dbg == f"hend{l}":
                d = dbg_tensor("hend", [NTOK, D], F32)
                P.dma("sp", d, h_scr, writes=["dbg"])
                raise _Stop()
    except _Stop:
        pass
    P.barrier()
    P.set_phase("end")
    build_program.phase_log = P.phase_log
    return nc, dbg_out


def _consts():
    ident = np.eye(128, dtype=np.float32)
    idx = np.arange(128)
    perm16 = np.zeros((128, 128), np.float32)
    perm16[idx ^ 16, idx] = 1.0
    perm32 = np.zeros((128, 128), np.float32)
    perm32[idx ^ 32, idx] = 1.0
    n = np.arange(SEQ)
    row = (n // GRID_W).astype(np.float32)
    col = (n % GRID_W).astype(np.float32)

    def tables(dim):
        half = dim // 2
        q = dim // 4
        freqs = (10000.0 ** (-np.arange(0, half, 2, dtype=np.float32) / half)).astype(np.float32)
        C = np.zeros((dim, SEQ), np.float32)
        S = np.zeros((dim, SEQ), np.float32)
        for i, pos in enumerate((row, col)):
            ang = (pos[None, :] * freqs[:, None]).astype(np.float32)
            for j in range(2):
                p0 = i * 2 * q + j * q
                C[p0:p0 + q] = np.cos(ang)
                S[p0:p0 + q] = np.sin(ang) * (-1.0 if j == 0 else 1.0)
        return C, S

    C128, S128 = tables(128)
    C64, S64 = tables(64)
    rope = np.stack([C128, S128, np.concatenate([C64, C64]), np.concatenate([S64, S64])]).astype(np.float32)
    kc = np.arange(64)[:, None]
    qc = np.arange(64)[None, :]
    cs = np.clip(qc - 8, 0, 64 - 16)
    cm = ((kc >= cs) & (kc < cs + 16)).astype(np.float32)
    cmask = np.concatenate([cm, cm], 0)
    return dict(c_ident=ident, c_perm16=perm16, c_perm32=perm32, c_rope=rope, c_cmask=cmask)


_CACHE = {}


def kernel(**inputs):
    dbg = inputs.pop("_dbg", None)
    ncores = inputs.pop("_ncores", 8)
    f = lambda k: np.ascontiguousarray(np.asarray(inputs[k], dtype=np.float32))
    key = ("prog", dbg)
    if key not in _CACHE:
        _CACHE[key] = build_program(dbg)
    nc, dbg_out = _CACHE[key]
    shared = {k: f(k) for k in ("w_mod", "b_mod", "g_mix", "w_in", "mla_g_q", "mla_w_qup", "mla_g_kv", "mla_w_kvup",
                                "gqa_g_q", "gqa_g_k", "diff_g_sub", "w_out", "g_ffn", "ffn_w_gate", "ffn_w_up",
                                "ffn_w_down", "moe_w_router", "moe_b_router", "moe_w_gate", "moe_w_up", "moe_w_down")}
    shared["g_final"] = f("g_final").reshape(1, D)
    shared["c_ctx"] = f("c_ctx").reshape(1, D)
    shared["diff_lam"] = np.ascontiguousarray(np.stack([f("diff_lq1"), f("diff_lk1"), f("diff_lq2"), f("diff_lk2")], axis=1))
    kc = np.arange(64)[:, None]
    qc = np.arange(64)[None, :]
    col_off = np.clip(kc - qc + 15, 0, 30)
    shared["na_bias_exp"] = np.ascontiguousarray(f("na_rel_bias")[:, :, :, col_off])
    shared.update(_consts())
    xs, cs, ctxs = f("x"), f("c"), f("ctx")
    in_maps = []
    for b in range(ncores):
        m = dict(shared)
        m["x"] = xs[b]
        m["c"] = cs[b:b + 1]
        m["ctx"] = ctxs[b]
        in_maps.append(m)
    res = run_bass_kernel_spmd(nc, in_maps, core_ids=list(range(ncores)))
    if dbg is not None:
        return res.results
    return np.stack([np.asarray(r["out"], dtype=np.float32) for r in res.results], axis=0)
```
